# Optimizing a Trainium2 kernel written in Bass

```python
import jax, jax.numpy as jnp
from jax import lax
import numpy as np

D_MODEL = 1024
BATCH = 8
SEQ = 4096
DEPTH = 2

CTX_LEN = 256
GRID_W = 64
A_CHUNK = 2 * GRID_W
A_WIDTH = D_MODEL
A_GROUPS = 8
A_GROUP_DIM = A_WIDTH // A_GROUPS
GLA_HEADS = 4
GLA_DK = D_MODEL // 2
GLA_DV = D_MODEL
GLA_HEAD_K = GLA_DK // GLA_HEADS
GLA_HEAD_V = GLA_DV // GLA_HEADS
GLA_GATE_RANK = 16
GLA_TAU = 16.0
GLA_CHUNK = 64
N_EXPERTS = 16
N_EXPERT_GROUPS = 4
EXPERTS_PER_GROUP = N_EXPERTS // N_EXPERT_GROUPS
TOP_K = 2
D_EXPERT = 3 * D_MODEL // 2
MOE_BLOCK = 128
EPS = 1e-6
IN_SIZES = (2 * A_WIDTH, GLA_DK, GLA_DK, GLA_DV, GLA_DV, GLA_GATE_RANK, GLA_GATE_RANK, 2 * D_MODEL)
IN_COLS = sum(IN_SIZES)

kernel_name = "hybrid_gmlp_gla_moe_diffusion_block"


def rms_norm(x, g):
    xf = x.astype(jnp.float32)
    y = xf * lax.rsqrt(jnp.mean(xf * xf, axis=-1, keepdims=True) + EPS)
    return (y * g.astype(jnp.float32)).astype(x.dtype)


def layer_norm(x, g, b):
    xf = x.astype(jnp.float32)
    mu = jnp.mean(xf, axis=-1, keepdims=True)
    xc = xf - mu
    y = xc * lax.rsqrt(jnp.mean(xc * xc, axis=-1, keepdims=True) + EPS)
    return (y * g.astype(jnp.float32) + b.astype(jnp.float32)).astype(x.dtype)


def split_cols(p):
    idx, acc = [], 0
    for s in IN_SIZES[:-1]:
        acc += s
        idx.append(acc)
    return jnp.split(p, idx, axis=-1)


def spatial_gating(z, ln_g, ln_b, w_s, b_s):
    u, v = jnp.split(z, 2, axis=-1)
    v = layer_norm(v, ln_g, ln_b)
    bsz, length, width = v.shape
    vr = v.reshape(bsz, length // A_CHUNK, A_CHUNK, A_GROUPS, A_GROUP_DIM)
    mixed = jnp.einsum('gts,bnsgc->bntgc', w_s, vr) + b_s.T[:, :, None]
    return u * mixed.reshape(bsz, length, width)


def gla_chunk_scan(q, k, v, log_a, s0):
    bsz, nh, length, _ = q.shape
    dv = v.shape[-1]
    n = length // GLA_CHUNK
    cs = lambda t: t.astype(jnp.float32).reshape(bsz, nh, n, GLA_CHUNK, t.shape[-1])
    q, k, v, log_a = cs(q), cs(k), cs(v), cs(log_a)
    b = jnp.cumsum(log_a, axis=-2)
    b_last = b[..., -1:, :]
    q_in = q * jnp.exp(b)
    k_in = k * jnp.exp(-b)
    k_st = k * jnp.exp(b_last - b)
    mask = jnp.tril(jnp.ones((GLA_CHUNK, GLA_CHUNK), dtype=bool))
    att = jnp.where(mask, jnp.einsum('bhntk,bhnsk->bhnts', q_in, k_in), 0.0)
    o_intra = jnp.einsum('bhnts,bhnsv->bhntv', att, v)

    def step(state, xs):
        qc, kc, vc, dc = xs
        o = jnp.einsum('bhtk,bhkv->bhtv', qc, state)
        state = dc[..., 0, :, None] * state + jnp.einsum('bhsk,bhsv->bhkv', kc, vc)
        return state, o

    xs = tuple(jnp.moveaxis(t, 2, 0) for t in (q_in, k_st, v, jnp.exp(b_last)))
    s_fin, o_inter = lax.scan(step, s0, xs)
    o = o_intra + jnp.moveaxis(o_inter, 0, 2)
    return o.reshape(bsz, nh, length, dv), s_fin


def gla_bidirectional(q, k, v, la_f, la_b, s_f, s_b):
    o_f, s_f = gla_chunk_scan(q, k, v, la_f, s_f)
    flip = lambda t: jnp.flip(t, axis=2)
    o_b, s_b = gla_chunk_scan(flip(q), flip(k), flip(v), flip(la_b), s_b)
    return o_f + flip(o_b), s_f, s_b


def token_mixer(h_x, h_c, w_in, w_gate2, b_gate2, gla_g, sgu_ln_g, sgu_ln_b, sgu_w, sgu_b,
                w_a, w_b, b_branch, w_out, with_ctx):
    parts_x = split_cols(h_x @ w_in)
    parts_c = split_cols(h_c @ w_in)

    def heads(t):
        bsz, length, width = t.shape
        return t.reshape(bsz, length, GLA_HEADS, width // GLA_HEADS).transpose(0, 2, 1, 3)

    def gla_inputs(parts):
        _, q, k, v, _, lr_f, lr_b, _ = parts
        la = [heads(jax.nn.log_sigmoid((lr @ w_gate2[d] + b_gate2[d]).astype(jnp.float32)) / GLA_TAU)
              for d, lr in enumerate((lr_f, lr_b))]
        return heads(q) * GLA_HEAD_K ** -0.5, heads(k), heads(v), la[0], la[1]

    zero = jnp.zeros((h_c.shape[0], GLA_HEADS, GLA_HEAD_K, GLA_HEAD_V), jnp.float32)
    o_c, s_f, s_b = gla_bidirectional(*gla_inputs(parts_c), zero, zero)
    o_x, _, _ = gla_bidirectional(*gla_inputs(parts_x), s_f, s_b)

    def merge(parts, o):
        z_a, _, _, _, r, _, _, gates = parts
        y_a = spatial_gating(jax.nn.gelu(z_a), sgu_ln_g, sgu_ln_b, sgu_w, sgu_b) @ w_a
        o_n = o * lax.rsqrt(jnp.mean(o * o, axis=-1, keepdims=True) + EPS)
        bsz, _, length, _ = o.shape
        o_n = o_n.transpose(0, 2, 1, 3).reshape(bsz, length, GLA_DV) * gla_g
        y_b = (o_n.astype(r.dtype) * jax.nn.silu(r)) @ w_b
        g_a, g_b = jnp.split(jax.nn.sigmoid(gates + b_branch), 2, axis=-1)
        return (g_a * y_a + g_b * y_b) @ w_out

    y_x = merge(parts_x, o_x)
    y_c = merge(parts_c, o_c) if with_ctx else None
    return y_x, y_c


def moe_ffn(h, w_router, b_router, w_g, w_u, w_d):
    n_tok, d = h.shape
    logits = h.astype(jnp.float32) @ w_router.astype(jnp.float32) + b_router.astype(jnp.float32)
    probs = jax.nn.softmax(logits, axis=-1).reshape(n_tok, N_EXPERT_GROUPS, EXPERTS_PER_GROUP)
    group_score = lax.top_k(probs, TOP_K)[0].sum(-1)
    g_sel = jnp.argmax(group_score, axis=-1)
    in_group = jnp.take_along_axis(probs, g_sel[:, None, None], axis=1)[:, 0]
    top_w, top_i = lax.top_k(in_group, TOP_K)
    weight = top_w / jnp.sum(top_w, axis=-1, keepdims=True)
    expert = (g_sel[:, None] * EXPERTS_PER_GROUP + top_i).reshape(-1).astype(jnp.int32)

    n_slot = n_tok * TOP_K
    token = jnp.repeat(jnp.arange(n_tok, dtype=jnp.int32), TOP_K)
    order = jnp.argsort(expert)
    e_sorted = expert[order]
    counts = jnp.bincount(expert, length=N_EXPERTS).astype(jnp.int32)
    starts = jnp.cumsum(counts) - counts
    padded = (counts + MOE_BLOCK - 1) // MOE_BLOCK * MOE_BLOCK
    p_ends = jnp.cumsum(padded)
    p_starts = p_ends - padded
    dest_sorted = p_starts[e_sorted] + jnp.arange(n_slot, dtype=jnp.int32) - starts[e_sorted]
    n_blocks = -(-n_slot // MOE_BLOCK) + N_EXPERTS
    buf_tok = jnp.full((n_blocks * MOE_BLOCK,), n_tok, jnp.int32).at[dest_sorted].set(token[order])
    block_e = jnp.minimum(jnp.searchsorted(p_ends, jnp.arange(n_blocks, dtype=jnp.int32) * MOE_BLOCK,
                                           side='right'), N_EXPERTS - 1)
    h_pad = jnp.concatenate([h, jnp.zeros((1, d), h.dtype)], axis=0)
    x_blk = h_pad[buf_tok].reshape(n_blocks, MOE_BLOCK, d)

    def expert_block(args):
        xb, e = args
        return (jax.nn.silu(xb @ w_g[e]) * (xb @ w_u[e])) @ w_d[e]

    y_blk = lax.map(expert_block, (x_blk, block_e)).reshape(-1, d)
    dest = jnp.zeros((n_slot,), jnp.int32).at[order].set(dest_sorted)
    y = y_blk[dest].reshape(n_tok, TOP_K, d) * weight[..., None].astype(h.dtype)
    return jnp.sum(y, axis=1)


def setup_inputs(seed: int = 0) -> dict:
    key = jax.random.key(seed)
    ks = iter(jax.random.split(key, 32))
    nrm = lambda shape, std: jax.random.normal(next(ks), shape, jnp.float32) * std
    L = DEPTH
    return {
        "x": nrm((BATCH, SEQ, D_MODEL), 1.0),
        "c": nrm((BATCH, D_MODEL), 1.0),
        "ctx": nrm((BATCH, CTX_LEN, D_MODEL), 1.0),
        "c_ctx": nrm((D_MODEL,), 1.0),
        "w_mod": nrm((L, D_MODEL, 6 * D_MODEL), 0.5 * D_MODEL ** -0.5),
        "b_mod": nrm((L, 6 * D_MODEL), 0.01),
        "g_norm1": 1.0 + nrm((L, D_MODEL), 0.01),
        "g_norm2": 1.0 + nrm((L, D_MODEL), 0.01),
        "w_in": nrm((L, D_MODEL, IN_COLS), D_MODEL ** -0.5),
        "w_gate2": nrm((L, 2, GLA_GATE_RANK, GLA_DK), GLA_GATE_RANK ** -0.5),
        "b_gate2": nrm((L, 2, GLA_DK), 0.1),
        "gla_norm_g": 1.0 + nrm((L, GLA_DV), 0.01),
        "sgu_ln_g": 1.0 + nrm((L, A_WIDTH), 0.01),
        "sgu_ln_b": nrm((L, A_WIDTH), 0.01),
        "sgu_w": nrm((L, A_GROUPS, A_CHUNK, A_CHUNK), A_CHUNK ** -0.5),
        "sgu_b": 1.0 + nrm((L, A_GROUPS, A_CHUNK), 0.01),
        "w_branch_a": nrm((L, A_WIDTH, D_MODEL), A_WIDTH ** -0.5),
        "w_branch_b": nrm((L, GLA_DV, D_MODEL), GLA_DV ** -0.5),
        "b_branch": nrm((L, 2 * D_MODEL), 0.01),
        "w_out": nrm((L, D_MODEL, D_MODEL), D_MODEL ** -0.5),
        "w_router": nrm((D_MODEL, N_EXPERTS), D_MODEL ** -0.5),
        "b_router": nrm((N_EXPERTS,), 0.01),
        "w_exp_gate": nrm((L, N_EXPERTS, D_MODEL, D_EXPERT), D_MODEL ** -0.5),
        "w_exp_up": nrm((L, N_EXPERTS, D_MODEL, D_EXPERT), D_MODEL ** -0.5),
        "w_exp_down": nrm((L, N_EXPERTS, D_EXPERT, D_MODEL), D_EXPERT ** -0.5),
        "g_final": 1.0 + nrm((D_MODEL,), 0.01),
    }


def reference(x, c, ctx, c_ctx, w_mod, b_mod, g_norm1, g_norm2, w_in, w_gate2, b_gate2, gla_norm_g,
              sgu_ln_g, sgu_ln_b, sgu_w, sgu_b, w_branch_a, w_branch_b, b_branch, w_out,
              w_router, b_router, w_exp_gate, w_exp_up, w_exp_down, g_final):
    bsz, seq, d = x.shape
    n_lat = bsz * seq
    for layer in range(DEPTH):
        with_ctx = layer < DEPTH - 1
        mod_x = (jax.nn.silu(c) @ w_mod[layer] + b_mod[layer])[:, None, :]
        mod_c = jax.nn.silu(c_ctx) @ w_mod[layer] + b_mod[layer]
        sh1, sc1, gt1, sh2, sc2, gt2 = jnp.split(mod_x, 6, axis=-1)
        csh1, csc1, cgt1, csh2, csc2, cgt2 = jnp.split(mod_c, 6, axis=-1)

        h_x = rms_norm(x, g_norm1[layer]) * (1.0 + sc1) + sh1
        h_c = rms_norm(ctx, g_norm1[layer]) * (1.0 + csc1) + csh1
        y_x, y_c = token_mixer(h_x, h_c, w_in[layer], w_gate2[layer], b_gate2[layer], gla_norm_g[layer],
                               sgu_ln_g[layer], sgu_ln_b[layer], sgu_w[layer], sgu_b[layer],
                               w_branch_a[layer], w_branch_b[layer], b_branch[layer], w_out[layer], with_ctx)
        x = x + gt1 * y_x

        tokens = (rms_norm(x, g_norm2[layer]) * (1.0 + sc2) + sh2).reshape(-1, d)
        if with_ctx:
            ctx = ctx + cgt1 * y_c
            h_c2 = rms_norm(ctx, g_norm2[layer]) * (1.0 + csc2) + csh2
            tokens = jnp.concatenate([tokens, h_c2.reshape(-1, d)], axis=0)
        y = moe_ffn(tokens, w_router, b_router, w_exp_gate[layer], w_exp_up[layer], w_exp_down[layer])
        x = x + gt2 * y[:n_lat].reshape(bsz, seq, d)
        if with_ctx:
            ctx = ctx + cgt2 * y[n_lat:].reshape(ctx.shape)
    return rms_norm(x, g_final)
```

```python
import contextlib
import numpy as np
import concourse.bass as bass
import concourse.mybir as mybir
from concourse.bass_utils import run_bass_kernel_spmd

F32 = mybir.dt.float32
BF16 = mybir.dt.bfloat16
AF = mybir.ActivationFunctionType
ALU = mybir.AluOpType
AX = mybir.AxisListType

D = 1024
INC = 7200
DE = 1536
NE = 16
EPS = 1e-6
PE, ACT, DVE, POOL, SP = "pe", "act", "dve", "pool", "sp"


class Buf:
    def __init__(self, name):
        self.name = name
        self.last_w = None
        self.readers = []
        self.dsem = None


class DSem:
    def __init__(self, sem):
        self.sem = sem
        self.total = 0


class K:
    def __init__(self, nc, stack):
        self.nc = nc
        self.stack = stack
        self.h = {PE: nc.tensor, ACT: nc.scalar, DVE: nc.vector, POOL: nc.gpsimd, SP: nc.sync}
        self.sem = {}
        self.cnt = {}
        for e in (PE, ACT, DVE, POOL):
            self.sem[e] = stack.enter_context(nc.semaphore("s_" + e))
            self.cnt[e] = 0
        self.seen = {e: {} for e in self.h}
        self.bufs = {}
        self.free_dsems = []
        self.all_dsems = []
        self.n_dsem = 0
        self.phase_bufs = None

    def _reg(self, name, t):
        b = Buf(name)
        self.bufs[name] = b
        if self.phase_bufs is not None:
            self.phase_bufs.append(name)
        return t

    def _uniq(self, name):
        self.n_names = getattr(self, "n_names", 0) + 1
        return "%s_u%d" % (name, self.n_names)

    def sb(self, name, shape, dt, stack=None):
        st = stack or self.cur_stack
        name = self._uniq(name)
        t = st.enter_context(self.nc.sbuf_tensor(name, list(shape), dt))
        return self._reg(name, t)

    def sbu(self, name, shape, dt):
        return self.cur_stack.enter_context(self.nc.sbuf_tensor(self._uniq(name), list(shape), dt))

    def ps(self, name, shape, dt, stack=None):
        st = stack or self.cur_stack
        name = self._uniq(name)
        t = st.enter_context(self.nc.psum_tensor(name, list(shape), dt))
        return self._reg(name, t)

    def get_dsem(self):
        if self.free_dsems:
            return self.free_dsems.pop()
        s = self.stack.enter_context(self.nc.semaphore("d%d" % self.n_dsem))
        self.n_dsem += 1
        ds = DSem(s)
        self.all_dsems.append(ds)
        return ds

    def _wait_tok(self, eng, tok):
        if tok[0] == "e":
            _, src, c = tok
            if src == eng and eng == PE:
                return
            if self.seen[eng].get(src, 0) >= c:
                return
            self.h[eng].wait_ge(self.sem[src], c)
            self.seen[eng][src] = c
        else:
            _, ds, v = tok
            v = max(v, ds.total)
            if self.seen[eng].get(ds, 0) >= v:
                return
            self.h[eng].wait_ge(ds.sem, v)
            self.seen[eng][ds] = v

    def _bufs_of(self, aps):
        out = []
        for a in aps:
            if a is None or isinstance(a, (int, float)):
                continue
            b = self.bufs.get(a.name)
            if b is not None and b not in out:
                out.append(b)
        return out

    def _deps(self, eng, rb, wb):
        for b in rb:
            if b.last_w is not None:
                self._wait_tok(eng, b.last_w)
        for b in wb:
            if b.last_w is not None:
                self._wait_tok(eng, b.last_w)
            for t in b.readers:
                if t[0] == "e" and t[1] == eng:
                    continue
                self._wait_tok(eng, t)

    def I(self, eng, fn, outs, ins):
        rb = self._bufs_of(ins)
        wb = self._bufs_of(outs)
        self._deps(eng, rb, wb)
        inst = fn(self.h[eng])
        self.cnt[eng] += 1
        inst.then_inc(self.sem[eng], 1)
        tok = ("e", eng, self.cnt[eng])
        for b in wb:
            b.last_w = tok
            b.readers = []
        for b in rb:
            if b not in wb:
                b.readers.append(tok)
        return inst

    def dma(self, q, out, in_):
        rb = self._bufs_of([in_])
        wb = self._bufs_of([out])
        self._deps(q, rb, wb)
        sb = (wb or rb)[0]
        if sb.dsem is None:
            sb.dsem = self.get_dsem()
        ds = sb.dsem
        self.h[q].dma_start(out=out, in_=in_).then_inc(ds.sem, 16)
        ds.total += 16
        tok = ("d", ds, ds.total)
        for b in wb:
            b.last_w = tok
            b.readers = []
        for b in rb:
            b.readers.append(tok)

    def barrier(self):
        for e in self.h:
            for src in (PE, ACT, DVE, POOL):
                if src != e and self.cnt[src] > 0:
                    self._wait_tok(e, ("e", src, self.cnt[src]))
            for ds in self.all_dsems:
                if ds.total > 0:
                    self._wait_tok(e, ("d", ds, ds.total))
        for e in (ACT, DVE, POOL):
            if self.cnt[e] > 0 and self.seen[e].get(e, 0) < self.cnt[e]:
                self.h[e].wait_ge(self.sem[e], self.cnt[e])
                self.seen[e][e] = self.cnt[e]

    @contextlib.contextmanager
    def phase(self):
        prev_stack = getattr(self, "cur_stack", None)
        prev_pb = self.phase_bufs
        with contextlib.ExitStack() as st:
            self.cur_stack = st
            self.phase_bufs = []
            yield
            self.barrier()
            for n in self.phase_bufs:
                b = self.bufs.pop(n)
                if b.dsem is not None:
                    self.free_dsems.append(b.dsem)
        self.cur_stack = prev_stack
        self.phase_bufs = prev_pb

    def mm(self, out, lhsT, rhs, start=True, stop=True):
        return self.I(PE, lambda e: e.matmul(out, lhsT, rhs, start=start, stop=stop, skip_group_check=True),
                      [out], [lhsT, rhs])

    def tr(self, out, in_, ident):
        return self.I(PE, lambda e: e.transpose(out, in_, ident), [out], [in_, ident])

    def act(self, eng_unused, out, in_, func, bias=None, scale=None, accum_out=None):
        kw = {}
        if bias is not None:
            kw["bias"] = bias
        if scale is not None:
            kw["scale"] = scale
        if accum_out is not None:
            kw["accum_out"] = accum_out
        return self.I(ACT, lambda e: e.activation(out=out, in_=in_, func=func, **kw), [out, accum_out],
                      [in_, bias, scale])

    def tt(self, eng, out, in0, in1, op):
        return self.I(eng, lambda e: e.tensor_tensor(out=out, in0=in0, in1=in1, op=op), [out], [in0, in1])

    def ts(self, eng, out, in0, s1, s2, op0, op1=None, accum_out=None):
        kw = {}
        if op1 is not None:
            kw["op1"] = op1
        if accum_out is not None:
            kw["accum_out"] = accum_out
        return self.I(eng, lambda e: e.tensor_scalar(out=out, in0=in0, scalar1=s1, scalar2=s2, op0=op0, **kw),
                      [out, accum_out], [in0, s1, s2])

    def stt(self, eng, out, in0, scalar, in1, op0, op1):
        return self.I(eng, lambda e: e.scalar_tensor_tensor(out=out, in0=in0, scalar=scalar, in1=in1, op0=op0, op1=op1),
                      [out], [in0, scalar, in1])

    def cp(self, eng, out, in_):
        if eng == ACT:
            return self.I(ACT, lambda e: e.copy(out=out, in_=in_), [out], [in_])
        return self.I(eng, lambda e: e.tensor_copy(out=out, in_=in_), [out], [in_])

    def memset(self, eng, ap, v):
        return self.I(eng, lambda e: e.memset(ap, v), [ap], [])

    def recip(self, out, in_):
        return self.I(DVE, lambda e: e.reciprocal(out=out, in_=in_), [out], [in_])

    def rmax(self, out, in_):
        return self.I(DVE, lambda e: e.reduce_max(out=out, in_=in_, axis=AX.X), [out], [in_])

    def rsum(self, out, in_):
        return self.I(DVE, lambda e: e.reduce_sum(out=out, in_=in_, axis=AX.X), [out], [in_])


def build(NTC=2, NTL=32, L=2, debug=(), NB=1):
    NT = NTC + NTL
    T = NT * 128
    nc = bass.Bass("TRN2", target_bir_lowering=False)

    def din(name, shape, dt=F32):
        return nc.dram_tensor(name, list(shape), dt, kind="ExternalInput").ap()

    dbg = set(debug)

    def dscr(name, shape, dt):
        kind = "ExternalOutput" if name in dbg else "Internal"
        return nc.dram_tensor(name, list(shape), dt, kind=kind).ap()

    xin_all = din("xin", [NB * T, D])
    cT_all = din("cT", [NB, 128, 8, 2])
    w_mod = din("w_mod", [L, D, 6 * D])
    b_mod = din("b_mod", [L, 6 * D])
    g_n1 = din("g_norm1", [L, D])
    g_n2 = din("g_norm2", [L, D])
    w_in = din("w_in", [L, D, INC])
    w_g2 = din("w_gate2", [L, 2, 16, 512])
    b_g2 = din("b_gate2", [L, 2, 512])
    gla_g = din("gla_norm_g", [L, D])
    ln_gT = din("ln_gT", [L, 128, 8])
    ln_b = din("sgu_ln_b", [L, D])
    sgu_wT = din("sgu_wT", [L, 8, 128, 128])
    sgu_b = din("sgu_b", [L, 8, 128])
    w_a = din("w_branch_a", [L, D, D])
    w_b = din("w_branch_b", [L, D, D])
    bbT = din("b_branchT", [L, 128, 16])
    w_out = din("w_out", [L, D, D])
    w_r = din("w_router", [D, NE])
    b_r = din("b_router", [1, NE])
    w_eg = din("w_exp_gate", [L, NE, D, DE])
    w_eu = din("w_exp_up", [L, NE, D, DE])
    w_ed = din("w_exp_down", [L, NE, DE, D])
    g_fin = din("g_final", [1, D])
    ident_d = din("ident", [128, 128])
    masks_d = din("masks", [4, 128, 128])
    out_all = nc.dram_tensor("out", [NB * NTL * 128, D], F32, kind="ExternalOutput").ap()

    xres = dscr("xres", [T, D], F32)
    modv = dscr("modv", [L, 2, 6 * D], F32)
    uT_d = dscr("uT", [8, 128, T], BF16)
    vg_d = dscr("vg", [T, D], BF16)
    gaT_d = dscr("gaT", [8, 128, T], BF16)
    gbT_d = dscr("gbT", [8, 128, T], BF16)
    yag_d = dscr("yagT", [8, 128, T], BF16)
    v_d = dscr("v_tok", [T, D], BF16)
    sr_d = dscr("sr", [T, D], BF16)
    qin_d = [dscr("qinT%d" % d, [4, 128, T], BF16) for d in range(2)]
    kin_d = [dscr("kinT%d" % d, [4, 128, T], BF16) for d in range(2)]
    kst_d = [dscr("kst%d" % d, [T, 512], BF16) for d in range(2)]
    ob_d = dscr("o_b", [T, D], F32)
    of_d = dscr("o_f", [T, D], F32)
    onT_d = dscr("onT", [8, 128, T], BF16)
    h2T_d = dscr("h2T", [8, 128, T], BF16)

    with contextlib.ExitStack() as top:
        k = K(nc, top)
        k.cur_stack = top
        ident = k.sb("ident", [128, 128], F32)
        identb = k.sb("identb", [128, 128], BF16)
        masks = k.sb("masks_sb", [128, 4, 128], F32)
        decay = k.sb("decay", [128, 2, NT, 8], F32)
        wts = k.sb("wts", [128, NT, NE], F32)
        brt = k.sb("brt", [128, NE], F32)
        wr = k.sb("wr", [128, 8, NE], F32)
        k.dma(SP, ident[:], ident_d[:, :])
        k.dma(SP, masks[:], masks_d.rearrange("m s t -> s m t"))
        k.dma(SP, brt[:], b_r.broadcast_to([128, NE]))
        k.dma(SP, wr[:], w_r.rearrange("(k p) n -> p k n", p=128))
        k.cp(DVE, identb[:], ident[:])
        triF, triFc, triB, triBc = (masks[:, i, :] for i in range(4))

        supers = []
        t0 = 0
        while t0 < NTC:
            n = min(4, NTC - t0)
            supers.append((t0, n))
            t0 += n
        while t0 < NT:
            n = min(4, NT - t0)
            supers.append((t0, n))
            t0 += n

        def rms_rstd(xt_ap, junk_ap, ss, rstd, n):
            k.act(ACT, junk_ap, xt_ap, AF.Square, accum_out=ss)
            k.act(ACT, rstd, ss, AF.Sqrt, bias=epsb[:, 0:1], scale=1.0 / n)
            k.recip(rstd, rstd)

        epsb = k.sb("epsb", [128, 1], F32)
        k.memset(DVE, epsb[:], EPS)
        oneb = k.sb("oneb", [128, 1], F32)
        k.memset(DVE, oneb[:], 1.0)

        for bl in range(NB * L):
            bi, l = divmod(bl, L)
            xin = xin_all[bi * T:(bi + 1) * T, :]
            cT = cT_all[bi]
            out = out_all[bi * NTL * 128:(bi + 1) * NTL * 128, :]
            last = l == L - 1
            xsrc = xin if l == 0 else xres
            with k.phase():
                cTs = k.sb("cTs", [128, 8, 2], F32)
                scT = k.sb("scT", [128, 8, 2], F32)
                k.dma(SP, cTs[:], cT[:, :, :])
                k.act(ACT, scT[:], cTs[:], AF.Silu)
                modsb = k.sb("modsb", [2, 6 * D], F32)
                bm2 = k.sb("bm2", [2, 6 * D], F32)
                gn = k.sb("gn", [2, 2, D], F32)
                k.dma(SP, bm2[:], b_mod[l:l + 1, :].broadcast_to([2, 6 * D]))
                k.dma(SP, gn[:, 0, :], g_n1[l:l + 1, :].broadcast_to([2, D]))
                k.dma(SP, gn[:, 1, :], g_n2[l:l + 1, :].broadcast_to([2, D]))
                wm = [k.sb("wm%d" % i, [128, 8, 512], F32) for i in range(2)]
                pm = [k.ps("pm%d" % i, [128, 512], F32) for i in range(2)]
                wmv = w_mod[l].rearrange("(k p) n -> p k n", p=128)
                for cc in range(12):
                    w_ = wm[cc % 2]
                    k.dma(SP, w_[:], wmv[:, :, cc * 512:(cc + 1) * 512])
                    p_ = pm[cc % 2]
                    for kk in range(8):
                        k.mm(p_[0:2, :], scT[:, kk, :], w_[:, kk, :], start=(kk == 0), stop=(kk == 7))
                    k.tt(DVE, modsb[:, cc * 512:(cc + 1) * 512], p_[0:2, :], bm2[:, cc * 512:(cc + 1) * 512], ALU.add)
                k.stt(DVE, modsb[:, D:2 * D], modsb[:, D:2 * D], 1.0, gn[:, 0, :], ALU.add, ALU.mult)
                k.stt(DVE, modsb[:, 4 * D:5 * D], modsb[:, 4 * D:5 * D], 1.0, gn[:, 1, :], ALU.add, ALU.mult)
                k.dma(SP, modv[l], modsb[:])

            def bc_load(dst, row, slot):
                k.dma(SP, dst, modv[l, row:row + 1, slot * D:(slot + 1) * D].broadcast_to([128, D]))

            with k.phase():
                wi = k.sb("wi", [128, 8, INC], BF16)
                wiv = w_in[l].rearrange("(k p) n -> p k n", p=128)
                for kk in range(8):
                    k.dma(POOL, wi[:, kk, :], wiv[:, kk, :])
                G1 = k.sb("G1", [128, D], F32)
                SH1 = k.sb("SH1", [128, D], F32)
                cur_var = [-1]
                wg2 = k.sb("wg2", [17, 2, 512], BF16)
                for d in range(2):
                    k.dma(POOL, wg2[0:16, d, :], w_g2[l, d])
                    k.dma(POOL, wg2[16:17, d, :], b_g2[l, d:d + 1, :])
                bb = k.sb("bb", [128, 16], F32)
                k.dma(SP, bb[:], bbT[l])
                xt = [k.sb("xt%d" % i, [128, D], F32) for i in range(2)]
                junk = k.sbu("junk", [128, D], BF16)
                tmp = k.sb("tmpA", [128, D], F32)
                hb = k.sb("hb", [128, D], BF16)
                ss = k.sb("ssA", [128, 1], F32)
                rstd = k.sb("rstdA", [128, 1], F32)
                hT = [k.sb("hT%d" % i, [128, 8, 512], BF16) for i in range(2)]
                pT = k.ps("pT", [128, D], BF16)
                pf = [k.ps("pf%d" % i, [128, 512], F32) for i in range(3)]
                pl = k.ps("pl", [16, 512], F32)
                pt = [k.ps("pt%d" % i, [128, 512], F32) for i in range(3)]
                stg = [k.sb("stg%d" % i, [128, 512], BF16) for i in range(4)]
                scnt = [0]
                qTs = k.sb("qTs", [128, 4, 512], BF16)
                kTs = k.sb("kTs", [128, 4, 512], BF16)
                lr1 = [k.sb("lr1_%d" % d, [32, 512], BF16) for d in range(2)]
                for d in range(2):
                    k.memset(DVE, lr1[d][:], 1.0)
                vgs = k.sb("vgs", [128, D], F32)
                vgb = k.sb("vgb", [128, D], BF16)
                vst = k.sb("vst", [128, 2], F32)
                vsb = k.sb("vsb", [128, D], BF16)
                srb = k.sb("srb", [128, D], BF16)
                ktk = k.sb("ktk", [128, 512], BF16)
                sp_ = k.sb("sp_", [128, 512], F32)
                ebt = k.sb("ebt", [128, 4, 128], F32)
                enbt = k.sb("enbt", [128, 4, 128], F32)
                qinb = k.sb("qinb", [128, 4, 128], BF16)
                kinb = k.sb("kinb", [128, 4, 128], BF16)
                edt = k.sb("edt", [128, 512], F32)
                kstb = k.sb("kstb", [128, 512], BF16)
                fcnt = [0]
                tcnt = [0]

                def nxt_pf():
                    fcnt[0] += 1
                    return pf[fcnt[0] % 3]

                def nxt_pt():
                    tcnt[0] += 1
                    return pt[tcnt[0] % 3]

                def xload(tj):
                    xb_ = xt[tj % 2]
                    k.dma(SP, xb_[:], xsrc[tj * 128:(tj + 1) * 128, :])
                    if l == 0:
                        k.dma(SP, xres[tj * 128:(tj + 1) * 128, :], xb_[:])

                for si, (ts0, nts) in enumerate(supers):
                    W = nts * 128
                    c0 = ts0 * 128
                    var = 1 if ts0 < NTC else 0
                    if cur_var[0] != var:
                        bc_load(G1[:], var, 1)
                        bc_load(SH1[:], var, 0)
                        cur_var[0] = var
                    hTs = hT[si % 2]
                    if si == 0:
                        xload(0)
                    for i in range(nts):
                        ti = ts0 + i
                        x_ = xt[ti % 2]
                        if ti + 1 < NT:
                            xload(ti + 1)
                        rms_rstd(x_[:], junk[:], ss[:], rstd[:], D)
                        k.stt(DVE, tmp[:], x_[:], rstd[:, 0:1], G1[:], ALU.mult, ALU.mult)
                        k.tt(POOL, hb[:], tmp[:], SH1[:], ALU.add)
                        for kk in range(8):
                            k.tr(pT[:, kk * 128:(kk + 1) * 128], hb[:, kk * 128:(kk + 1) * 128], identb[:])
                        k.cp(ACT, hTs[:, :, i * 128:(i + 1) * 128], pT[:].rearrange("p (k t) -> p k t", k=8))

                    def fm(col0, M):
                        p_ = nxt_pf() if M == 128 else pl
                        for kk in range(8):
                            k.mm(p_[0:M, 0:W], wi[:, kk, col0:col0 + M], hTs[:, kk, 0:W], start=(kk == 0), stop=(kk == 7))
                        return p_

                    def nxt_stg():
                        scnt[0] += 1
                        return stg[scnt[0] % 4]

                    for j in range(8):
                        p_ = fm(j * 128, 128)
                        s_ = nxt_stg()
                        k.act(ACT, s_[:, 0:W], p_[:, 0:W], AF.Gelu_apprx_tanh)
                        k.dma(SP, uT_d[j, :, c0:c0 + W], s_[:, 0:W])
                    for h in range(4):
                        p_ = fm(2048 + h * 128, 128)
                        k.act(ACT, qTs[:, h, 0:W], p_[:, 0:W], AF.Copy, scale=128.0 ** -0.5)
                        p_ = fm(2560 + h * 128, 128)
                        k.cp(DVE, kTs[:, h, 0:W], p_[:, 0:W])
                    for d in range(2):
                        p_ = fm(5120 + d * 16, 16)
                        k.cp(DVE, lr1[d][0:16, 0:W], p_[0:16, 0:W])
                    for j in range(16):
                        p_ = fm(5152 + j * 128, 128)
                        dst = gaT_d if j < 8 else gbT_d
                        s_ = nxt_stg()
                        k.act(ACT, s_[:, 0:W], p_[:, 0:W], AF.Sigmoid, bias=bb[:, j:j + 1])
                        k.dma(SP, dst[j % 8, :, c0:c0 + W], s_[:, 0:W])

                    for i in range(nts):
                        ti = ts0 + i
                        r0 = ti * 128
                        tsl = slice(i * 128, (i + 1) * 128)

                        def tm(col0, N):
                            p_ = nxt_pt()
                            for kk in range(8):
                                k.mm(p_[:, 0:N], hTs[:, kk, tsl], wi[:, kk, col0:col0 + N], start=(kk == 0), stop=(kk == 7))
                            return p_

                        for hh in range(2):
                            p_ = tm(1024 + hh * 512, 512)
                            k.act(ACT, vgs[:, hh * 512:(hh + 1) * 512], p_[:, :], AF.Gelu_apprx_tanh)
                        k.rsum(vst[:, 0:1], vgs[:])
                        k.ts(DVE, vst[:, 0:1], vst[:, 0:1], -1.0 / D, None, ALU.mult)
                        k.act(ACT, junk[:], vgs[:], AF.Square, bias=vst[:, 0:1], accum_out=vst[:, 1:2])
                        k.act(ACT, vst[:, 1:2], vst[:, 1:2], AF.Sqrt, bias=epsb[:, 0:1], scale=1.0 / D)
                        k.recip(vst[:, 1:2], vst[:, 1:2])
                        k.ts(DVE, vgb[:], vgs[:], vst[:, 0:1], vst[:, 1:2], ALU.add, ALU.mult)
                        k.dma(SP, vg_d[r0:r0 + 128, :], vgb[:])
                        p_ = tm(2560, 512)
                        k.cp(ACT, ktk[:], p_[:, :])
                        for hh in range(2):
                            p_ = tm(3072 + hh * 512, 512)
                            k.cp(DVE if hh else ACT, vsb[:, hh * 512:(hh + 1) * 512], p_[:, :])
                        k.dma(SP, v_d[r0:r0 + 128, :], vsb[:])
                        for hh in range(2):
                            p_ = tm(4096 + hh * 512, 512)
                            k.act(ACT, srb[:, hh * 512:(hh + 1) * 512], p_[:, :], AF.Silu)
                        k.dma(SP, sr_d[r0:r0 + 128, :], srb[:])
                        for d in range(2):
                            tri = triF if d == 0 else triB
                            tric = triFc if d == 0 else triBc
                            p_ = nxt_pt()
                            k.mm(p_[:, :], lr1[d][0:17, tsl], wg2[0:17, d, :])
                            k.act(ACT, sp_[:], p_[:, :], AF.Exp, scale=-1.0)
                            k.act(ACT, sp_[:], sp_[:], AF.Ln, bias=oneb[:, 0:1])
                            p2 = nxt_pt()
                            for h in range(4):
                                k.mm(p2[:, h * 128:(h + 1) * 128], sp_[:, h * 128:(h + 1) * 128], tri)
                            p2v = p2[:, :].rearrange("p (h t) -> p h t", h=4)
                            k.act(ACT, ebt[:], p2v, AF.Exp, scale=-1.0 / 16)
                            k.act(ACT, enbt[:], p2v, AF.Exp, scale=1.0 / 16)
                            k.tt(DVE, qinb[:], qTs[:, :, tsl], ebt[:], ALU.mult)
                            k.tt(DVE, kinb[:], kTs[:, :, tsl], enbt[:], ALU.mult)
                            k.dma(SP, qin_d[d][:, :, r0:r0 + 128].rearrange("h p t -> p h t"), qinb[:])
                            k.dma(SP, kin_d[d][:, :, r0:r0 + 128].rearrange("h p t -> p h t"), kinb[:])
                            cs = slice(63, 128, 64) if d == 0 else slice(0, 128, 64)
                            k.cp(DVE, decay[:, d, ti, :].rearrange("p (h c) -> p h c", h=4), ebt[:, :, cs])
                            p3 = nxt_pt()
                            k.mm(p3[:, :], tric, sp_[:])
                            k.act(ACT, edt[:], p3[:, :], AF.Exp, scale=-1.0 / 16)
                            k.tt(DVE, kstb[:], ktk[:], edt[:], ALU.mult)
                            k.dma(SP, kst_d[d][r0:r0 + 128, :], kstb[:])

            with k.phase():
                wa = k.sb("wa", [128, 8, D], BF16)
                k.dma(POOL, wa[:], w_a[l].rearrange("(k p) n -> p k n", p=128))
                wsT = k.sb("wsT", [128, 8, 128], BF16)
                k.dma(POOL, wsT[:], sgu_wT[l].rearrange("g s t -> s g t"))
                wsT32 = k.sb("wsT32", [128, 8, 128], F32)
                k.dma(SP, wsT32[:], sgu_wT[l].rearrange("g s t -> s g t"))
                lng = k.sb("lng", [128, 8], F32)
                k.dma(SP, lng[:], ln_gT[l])
                ones32 = k.sb("ones32", [128, 128], F32)
                k.memset(DVE, ones32[:], 1.0)
                R2 = k.sb("R2", [2, 8, 128], F32)
                L2 = k.sb("L2", [2, D], F32)
                k.memset(DVE, L2[:], 1.0)
                k.dma(SP, L2[0:1, :], ln_b[l:l + 1, :])
                k.dma(SP, R2[1:2, :, :], sgu_b[l:l + 1, :, :])
                pmx = [k.ps("pmx%d" % i, [128, D], F32) for i in range(2)]
                prs, pbs = pmx
                for g in range(8):
                    k.mm(prs[0:1, g * 128:(g + 1) * 128], ones32[:, 0:1], wsT32[:, g, :])
                k.cp(DVE, R2[0:1, :, :], prs[0:1, :].rearrange("p (g t) -> p g t", g=8))
                for g in range(8):
                    k.mm(pbs[:, g * 128:(g + 1) * 128], L2[0:2, g * 128:(g + 1) * 128], R2[0:2, g, :])
                Bias = k.sb("Bias", [128, 8, 128], F32)
                k.cp(DVE, Bias[:], pbs[:, :].rearrange("p (g t) -> p g t", g=8))
                vgt = [k.sb("vgt%d" % i, [128, D], BF16) for i in range(2)]
                uTl = [k.sb("uTl%d" % i, [128, 8, 512], BF16) for i in range(2)]
                gaTl = [k.sb("gaTl%d" % i, [128, 8, 512], BF16) for i in range(2)]
                tmpB = k.sb("tmpB", [128, 8, 128], F32)
                gT = k.sb("gT", [128, 8, 512], BF16)
                py = [k.ps("py%d" % i, [128, 512], F32) for i in range(2)]
                yag = k.sb("yag", [128, 8, 512], BF16)
                def sloadB(sj):
                    tsj, ntj = supers[sj]
                    Wj = ntj * 128
                    cj = tsj * 128
                    k.dma(SP, uTl[sj % 2][:, :, 0:Wj], uT_d[:, :, cj:cj + Wj].rearrange("j p t -> p j t"))
                    k.dma(SP, gaTl[sj % 2][:, :, 0:Wj], gaT_d[:, :, cj:cj + Wj].rearrange("j p t -> p j t"))

                def vloadB(tj):
                    k.dma(SP, vgt[tj % 2][:], vg_d[tj * 128:(tj + 1) * 128, :])

                sloadB(0)
                vloadB(0)
                for si, (ts0, nts) in enumerate(supers):
                    W = nts * 128
                    c0 = ts0 * 128
                    uT_ = uTl[si % 2]
                    ga_ = gaTl[si % 2]
                    if si + 1 < len(supers):
                        sloadB(si + 1)
                    for i in range(nts):
                        ti = ts0 + i
                        v_ = vgt[ti % 2]
                        if ti + 1 < NT:
                            vloadB(ti + 1)
                        pm_ = pmx[ti % 2]
                        for g in range(8):
                            k.mm(pm_[:, g * 128:(g + 1) * 128], v_[:, g * 128:(g + 1) * 128], wsT[:, g, :])
                        pv = pm_[:, :].rearrange("p (g t) -> p g t", g=8)
                        k.tt(DVE, tmpB[:], pv, lng[:, :].unsqueeze(2).broadcast_to([128, 8, 128]), ALU.mult)
                        k.tt(POOL, tmpB[:], tmpB[:], Bias[:], ALU.add)
                        k.tt(DVE, gT[:, :, i * 128:(i + 1) * 128], tmpB[:], uT_[:, :, i * 128:(i + 1) * 128], ALU.mult)
                    for oc in range(8):
                        p_ = py[oc % 2]
                        for kk in range(8):
                            k.mm(p_[:, 0:W], wa[:, kk, oc * 128:(oc + 1) * 128], gT[:, kk, 0:W], start=(kk == 0), stop=(kk == 7))
                        k.tt(DVE, yag[:, oc, 0:W], p_[:, 0:W], ga_[:, oc, 0:W], ALU.mult)
                    k.dma(SP, yag_d[:, :, c0:c0 + W].rearrange("j p t -> p j t"), yag[:, :, 0:W])

            with k.phase():
                zer = k.sb("zer", [128, 128], BF16)
                k.memset(DVE, zer[:], 0.0)
                DS = {}
                for d in (1, 0):
                    st = {}
                    if d == 1:
                        st["order"] = list(range(NTC - 1, -1, -1)) + list(range(NT - 1, NTC - 1, -1))
                        st["co"] = (1, 0)
                        st["msk"] = triB
                    else:
                        st["order"] = list(range(NT))
                        st["co"] = (0, 1)
                        st["msk"] = triF
                    st["S32"] = [k.sb("S32_%d_%d" % (d, h), [128, 256], F32) for h in range(4)]
                    st["S16"] = [k.sb("S16_%d_%d" % (d, h), [128, 256], BF16) for h in range(4)]
                    for h in range(4):
                        k.memset(DVE, st["S32"][h][:], 0.0)
                        k.memset(POOL, st["S16"][h][:], 0.0)
                    st["qn"] = [k.sb("qn%d_%d" % (d, i), [128, 4, 128], BF16) for i in range(2)]
                    st["kn"] = [k.sb("kn%d_%d" % (d, i), [128, 4, 128], BF16) for i in range(2)]
                    st["ks"] = [k.sb("ks%d_%d" % (d, i), [128, 512], BF16) for i in range(2)]
                    st["vv"] = [k.sb("vv%d_%d" % (d, i), [128, D], BF16) for i in range(2)]
                    st["patt"] = k.ps("patt%d" % d, [128, 512], F32)
                    st["po"] = k.ps("po%d" % d, [128, D], F32)
                    st["pkv"] = k.ps("pkv%d" % d, [128, 512], F32)
                    st["att"] = k.sb("att%d" % d, [128, 4, 128], BF16)
                    st["osb"] = [k.sb("osb%d_%d" % (d, i), [128, D], F32) for i in range(2)]
                    st["odst"] = ob_d if d == 1 else of_d
                    DS[d] = st

                def loadsC(d, ii):
                    st = DS[d]
                    ti = st["order"][ii]
                    r0 = ti * 128
                    b_ = ii % 2
                    k.dma(SP, st["qn"][b_][:], qin_d[d][:, :, r0:r0 + 128].rearrange("h p t -> p h t"))
                    k.dma(SP, st["kn"][b_][:], kin_d[d][:, :, r0:r0 + 128].rearrange("h p t -> p h t"))
                    k.dma(SP, st["ks"][b_][:], kst_d[d][r0:r0 + 128, :])
                    k.dma(SP, st["vv"][b_][:], v_d[r0:r0 + 128, :])

                def stage0(d, ii):
                    st = DS[d]
                    b_ = ii % 2
                    q_, k_, v_ = st["qn"][b_], st["kn"][b_], st["vv"][b_]
                    pa, po_, at_ = st["patt"], st["po"], st["att"]
                    for h in range(4):
                        k.mm(pa[:, h * 128:(h + 1) * 128], k_[:, h, :], q_[:, h, :])
                    k.tt(DVE, at_[:], pa[:, :].rearrange("p (h t) -> p h t", h=4),
                         st["msk"].unsqueeze(1).broadcast_to([128, 4, 128]), ALU.mult)
                    for hh in range(2):
                        k.mm(po_[:, hh * 512:(hh + 1) * 512], zer[:], v_[:, hh * 512:(hh + 1) * 512], start=True, stop=False)
                    for h in range(4):
                        k.mm(po_[:, h * 256:(h + 1) * 256], at_[:, h, :], v_[:, h * 256:(h + 1) * 256], start=False, stop=False)

                def stage_chunk(d, ii, ci):
                    st = DS[d]
                    b_ = ii % 2
                    ti = st["order"][ii]
                    c = st["co"][ci]
                    q_, ks_, v_ = st["qn"][b_], st["ks"][b_], st["vv"][b_]
                    po_, pkv, S32, S16 = st["po"], st["pkv"], st["S32"], st["S16"]
                    rs = slice(c * 64, (c + 1) * 64)
                    for h in range(4):
                        k.mm(po_[rs, h * 256:(h + 1) * 256], q_[:, h, rs], S16[h][:], start=False, stop=(ci == 1))
                    for hp in range(2):
                        for h in (2 * hp, 2 * hp + 1):
                            k.mm(pkv[:, (h % 2) * 256:(h % 2 + 1) * 256], ks_[rs, h * 128:(h + 1) * 128], v_[rs, h * 256:(h + 1) * 256])
                        for h in (2 * hp, 2 * hp + 1):
                            k.stt(DVE, S32[h][:], S32[h][:], decay[:, d, ti, h * 2 + c:h * 2 + c + 1],
                                  pkv[:, (h % 2) * 256:(h % 2 + 1) * 256], ALU.mult, ALU.add)
                            k.cp(ACT, S16[h][:], S32[h][:])

                def stage3(d, ii):
                    st = DS[d]
                    b_ = ii % 2
                    ti = st["order"][ii]
                    r0 = ti * 128
                    o_ = st["osb"][b_]
                    po_ = st["po"]
                    k.cp(ACT, o_[:, 0:512], po_[:, 0:512])
                    k.cp(DVE, o_[:, 512:D], po_[:, 512:D])
                    k.dma(SP, st["odst"][r0:r0 + 128, :], o_[:])

                for d in (1, 0):
                    loadsC(d, 0)
                for ii in range(NT):
                    for d in (1, 0):
                        if ii + 1 < NT:
                            loadsC(d, ii + 1)
                    for d in (1, 0):
                        stage0(d, ii)
                    for ci in range(2):
                        for d in (1, 0):
                            stage_chunk(d, ii, ci)
                    for d in (1, 0):
                        stage3(d, ii)

            with k.phase():
                GG = k.sb("GG", [128, D], F32)
                k.dma(SP, GG[:], gla_g[l:l + 1, :].broadcast_to([128, D]))
                ofl = [k.sb("ofl%d" % i, [128, D], F32) for i in range(2)]
                obl = [k.sb("obl%d" % i, [128, D], F32) for i in range(2)]
                srl = [k.sb("srl%d" % i, [128, D], BF16) for i in range(2)]
                osum = [k.sb("osum%d" % i, [128, D], F32) for i in range(2)]
                on32 = [k.sb("on32_%d" % i, [128, D], F32) for i in range(2)]
                onb = [k.sb("onb%d" % i, [128, D], BF16) for i in range(2)]
                junkc = k.sbu("junkc", [128, 256], BF16)
                ssc = [k.sb("ssc%d" % i, [128, 4], F32) for i in range(2)]
                pTc = [k.ps("pTc%d" % i, [128, D], BF16) for i in range(2)]
                onTs = [k.sb("onTs%d" % i, [128, 8, 128], BF16) for i in range(2)]
                tilesC = list(range(NTC, NT)) if last else list(range(NT))

                def loadC2(pos):
                    tj = tilesC[pos]
                    rj = tj * 128
                    k.dma(SP, ofl[pos % 2][:], of_d[rj:rj + 128, :])
                    k.dma(SP, obl[pos % 2][:], ob_d[rj:rj + 128, :])
                    k.dma(SP, srl[pos % 2][:], sr_d[rj:rj + 128, :])

                loadC2(0)
                for pos, ti in enumerate(tilesC):
                    b_ = pos % 2
                    r0 = ti * 128
                    if pos + 1 < len(tilesC):
                        loadC2(pos + 1)
                    o_ = osum[b_]
                    k.tt(DVE, o_[:], ofl[b_][:], obl[b_][:], ALU.add)
                    for h in range(4):
                        k.act(ACT, junkc[:], o_[:, h * 256:(h + 1) * 256], AF.Square, accum_out=ssc[b_][:, h:h + 1])
                    k.act(ACT, ssc[b_][:], ssc[b_][:], AF.Sqrt, bias=epsb[:, 0:1], scale=1.0 / 256)
                    k.recip(ssc[b_][:], ssc[b_][:])
                    k.tt(DVE, on32[b_][:].rearrange("p (h v) -> p h v", h=4), o_[:].rearrange("p (h v) -> p h v", h=4),
                         ssc[b_][:, :].unsqueeze(2).broadcast_to([128, 4, 256]), ALU.mult)
                    k.tt(DVE, on32[b_][:], on32[b_][:], GG[:], ALU.mult)
                    k.tt(POOL, onb[b_][:], on32[b_][:], srl[b_][:], ALU.mult)
                    for kk in range(8):
                        k.tr(pTc[b_][:, kk * 128:(kk + 1) * 128], onb[b_][:, kk * 128:(kk + 1) * 128], identb[:])
                    k.cp(ACT, onTs[b_][:], pTc[b_][:, :].rearrange("p (k t) -> p k t", k=8))
                    k.dma(SP, onT_d[:, :, r0:r0 + 128].rearrange("j p t -> p j t"), onTs[b_][:])

            with k.phase():
                wb_ = k.sb("wb_", [128, 8, D], BF16)
                wo_ = k.sb("wo_", [128, 8, D], BF16)
                k.dma(POOL, wb_[:], w_b[l].rearrange("(k p) n -> p k n", p=128))
                k.dma(POOL, wo_[:], w_out[l].rearrange("(k p) n -> p k n", p=128))
                GT1 = k.sb("GT1", [128, 2, D], F32)
                G2 = k.sb("G2", [128, 2, D], F32)
                SH2 = k.sb("SH2", [128, 2, D], F32)
                for r in range(2):
                    bc_load(GT1[:, r, :], r, 2)
                    bc_load(G2[:, r, :], r, 4)
                    bc_load(SH2[:, r, :], r, 3)
                onl = [k.sb("onl%d" % i, [128, 8, 512], BF16) for i in range(2)]
                yal = [k.sb("yal%d" % i, [128, 8, 512], BF16) for i in range(2)]
                gbl = [k.sb("gbl%d" % i, [128, 8, 512], BF16) for i in range(2)]
                pyb = [k.ps("pyb%d" % i, [128, 512], F32) for i in range(2)]
                tmpD = k.sb("tmpD", [128, 512], F32)
                mT = k.sb("mT", [128, 8, 512], BF16)
                xl = [k.sb("xl%d" % i, [128, D], F32) for i in range(2)]
                pyo = k.ps("pyo", [128, D], F32)
                tmpx = k.sb("tmpx", [128, D], F32)
                xn = [k.sb("xn%d" % i, [128, D], F32) for i in range(2)]
                junkd = k.sbu("junkd", [128, D], F32)
                ssd = k.sb("ssd", [128, 1], F32)
                rsd = k.sb("rsd", [128, 1], F32)
                h2 = k.sb("h2", [128, D], F32)
                pT32 = k.ps("pT32", [128, D], F32)
                h2T32 = k.sb("h2T32", [128, 8, 128], F32)
                h2Ts = [k.sb("h2Ts%d" % i, [128, 8, 512], BF16) for i in range(2)]
                plg = k.ps("plg", [128, NE], F32)
                lg = k.sb("lg", [128, NE], F32)
                rt = k.sb("rt", [128, 8], F32)
                pr = k.sb("pr", [128, NE], F32)
                p2_ = k.sb("p2_", [128, NE], F32)
                eq = k.sb("eq", [128, NE], F32)
                m1 = k.sb("m1", [128, 4], F32)
                m2 = k.sb("m2", [128, 4], F32)
                gs = k.sb("gs", [128, 4], F32)
                gmk = k.sb("gmk", [128, 4], F32)
                sel = k.sb("sel", [128, NE], F32)

                def g4(ap):
                    return ap.rearrange("p (g e) -> p g e", g=4)

                def b4(ap):
                    return ap.unsqueeze(2).broadcast_to([128, 4, 4])

                supD = [sp for sp in supers if not (last and sp[0] < NTC)]
                tilesD = [ts_ + i_ for (ts_, n_) in supD for i_ in range(n_)]

                def sloadD(sj):
                    tsj, ntj = supD[sj]
                    Wj = ntj * 128
                    cj = tsj * 128
                    k.dma(SP, onl[sj % 2][:, :, 0:Wj], onT_d[:, :, cj:cj + Wj].rearrange("j p t -> p j t"))
                    k.dma(SP, yal[sj % 2][:, :, 0:Wj], yag_d[:, :, cj:cj + Wj].rearrange("j p t -> p j t"))
                    k.dma(SP, gbl[sj % 2][:, :, 0:Wj], gbT_d[:, :, cj:cj + Wj].rearrange("j p t -> p j t"))

                def xloadD(tpos):
                    tj = tilesD[tpos]
                    k.dma(SP, xl[tpos % 2][:], xres[tj * 128:(tj + 1) * 128, :])

                sloadD(0)
                xloadD(0)
                tposD = 0
                for si, (ts0, nts) in enumerate(supD):
                    W = nts * 128
                    c0 = ts0 * 128
                    var = 1 if ts0 < NTC else 0
                    on_, ya_, gb_ = onl[si % 2], yal[si % 2], gbl[si % 2]
                    if si + 1 < len(supD):
                        sloadD(si + 1)
                    for oc in range(8):
                        p_ = pyb[oc % 2]
                        for kk in range(8):
                            k.mm(p_[:, 0:W], wb_[:, kk, oc * 128:(oc + 1) * 128], on_[:, kk, 0:W], start=(kk == 0), stop=(kk == 7))
                        k.tt(DVE, tmpD[:, 0:W], p_[:, 0:W], gb_[:, oc, 0:W], ALU.mult)
                        k.tt(POOL, mT[:, oc, 0:W], tmpD[:, 0:W], ya_[:, oc, 0:W], ALU.add)
                    h2T_ = h2Ts[si % 2]
                    for i in range(nts):
                        ti = ts0 + i
                        r0 = ti * 128
                        x_ = xl[tposD % 2]
                        xn_ = xn[tposD % 2]
                        tposD += 1
                        if tposD < len(tilesD):
                            xloadD(tposD)
                        for hh in range(2):
                            for kk in range(8):
                                k.mm(pyo[:, hh * 512:(hh + 1) * 512], mT[:, kk, i * 128:(i + 1) * 128],
                                     wo_[:, kk, hh * 512:(hh + 1) * 512], start=(kk == 0), stop=(kk == 7))
                        k.tt(DVE, tmpx[:], pyo[:, :], GT1[:, var, :], ALU.mult)
                        k.tt(POOL, xn_[:], tmpx[:], x_[:], ALU.add)
                        k.dma(SP, xres[r0:r0 + 128, :], xn_[:])
                        rms_rstd(xn_[:], junkd[:], ssd[:], rsd[:], D)
                        k.stt(DVE, tmpx[:], xn_[:], rsd[:, 0:1], G2[:, var, :], ALU.mult, ALU.mult)
                        k.tt(POOL, h2[:], tmpx[:], SH2[:, var, :], ALU.add)
                        for kk in range(8):
                            k.tr(pT32[:, kk * 128:(kk + 1) * 128], h2[:, kk * 128:(kk + 1) * 128], ident[:])
                        k.cp(ACT, h2T32[:], pT32[:, :].rearrange("p (k t) -> p k t", k=8))
                        k.cp(DVE, h2T_[:, :, i * 128:(i + 1) * 128], h2T32[:])
                        for kk in range(8):
                            k.mm(plg[:, :], h2T32[:, kk, :], wr[:, kk, :], start=(kk == 0), stop=(kk == 7))
                        k.tt(DVE, lg[:], plg[:, :], brt[:], ALU.add)
                        k.rmax(rt[:, 0:1], lg[:])
                        k.ts(DVE, rt[:, 1:2], rt[:, 0:1], -1.0, None, ALU.mult)
                        k.act(ACT, pr[:], lg[:], AF.Exp, bias=rt[:, 1:2], accum_out=rt[:, 2:3])
                        k.recip(rt[:, 3:4], rt[:, 2:3])
                        k.ts(DVE, pr[:], pr[:], rt[:, 3:4], None, ALU.mult)
                        k.I(DVE, lambda e: e.tensor_reduce(out=m1[:], in_=g4(pr[:]), axis=AX.X, op=ALU.max), [m1[:]], [pr[:]])
                        k.tt(DVE, g4(eq[:]), g4(pr[:]), b4(m1[:]), ALU.is_ge)
                        k.stt(DVE, p2_[:], eq[:], -4.0, pr[:], ALU.mult, ALU.add)
                        k.I(DVE, lambda e: e.tensor_reduce(out=m2[:], in_=g4(p2_[:]), axis=AX.X, op=ALU.max), [m2[:]], [p2_[:]])
                        k.tt(DVE, gs[:], m1[:], m2[:], ALU.add)
                        k.rmax(rt[:, 4:5], gs[:])
                        k.ts(DVE, gmk[:], gs[:], rt[:, 4:5], None, ALU.is_ge)
                        k.tt(DVE, g4(sel[:]), g4(pr[:]), b4(m2[:]), ALU.is_ge)
                        k.tt(DVE, g4(sel[:]), g4(sel[:]), b4(gmk[:]), ALU.mult)
                        k.tt(DVE, sel[:], sel[:], pr[:], ALU.mult)
                        k.rsum(rt[:, 5:6], sel[:])
                        k.recip(rt[:, 6:7], rt[:, 5:6])
                        k.ts(DVE, wts[:, ti, :], sel[:], rt[:, 6:7], None, ALU.mult)
                    k.dma(SP, h2T_d[:, :, c0:c0 + W].rearrange("j p t -> p j t"), h2T_[:, :, 0:W])

            moe_tiles = list(range(NTC, NT)) if last else list(range(NT))
            GSZ = 17
            groups = [moe_tiles[i:i + GSZ] for i in range(0, len(moe_tiles), GSZ)]
            with k.phase():
                GT2 = k.sb("GT2", [128, 2, D], F32)
                for r in range(2):
                    bc_load(GT2[:, r, :], r, 5)
                if last:
                    GF = k.sb("GF", [128, D], F32)
                    k.dma(SP, GF[:], g_fin.broadcast_to([128, D]))
                hT2 = k.sb("hT2", [128, 8, GSZ * 128], BF16)
                yacc = k.sb("yacc", [128, GSZ, D], F32)
                wg_ = [k.sb("wg_%d" % i, [128, 8, 512], BF16) for i in range(2)]
                wu_ = [k.sb("wu_%d" % i, [128, 8, 512], BF16) for i in range(2)]
                wd_ = [k.sb("wd_%d" % i, [128, 4, D], BF16) for i in range(2)]
                pg = [k.ps("pg%d" % i, [128, 512], F32) for i in range(2)]
                pu = [k.ps("pu%d" % i, [128, 512], F32) for i in range(2)]
                pd = [k.ps("pd%d" % i, [128, 512], F32) for i in range(4)]
                sg = [k.sb("sg%d" % i, [128, 512], F32) for i in range(2)]
                aT = [k.sb("aT%d" % i, [128, 4, 512], BF16) for i in range(2)]
                xe = [k.sb("xe%d" % i, [128, D], F32) for i in range(2)]
                tmpe = k.sb("tmpe", [128, D], F32)
                junke = k.sbu("junke", [128, D], F32)
                sse = k.sb("sse", [128, 1], F32)
                rse = k.sb("rse", [128, 1], F32)
                ucnt = 0
                dcnt = 0
                for grp in groups:
                    ng = len(grp)
                    g0 = grp[0] * 128
                    WG = ng * 128
                    k.dma(SP, hT2[:, :, 0:WG], h2T_d[:, :, g0:g0 + WG].rearrange("j p t -> p j t"))
                    k.memset(POOL, yacc[:, 0:ng, :], 0.0)
                    gsup = [(s, min(4, ng - s)) for s in range(0, ng, 4)]
                    units = [(e, q) for e in range(NE) for q in range(3)]

                    def load_unit(ui, buf):
                        e, q = units[ui]
                        k.dma(POOL, wg_[buf][:], w_eg[l, e].rearrange("(k p) n -> p k n", p=128)[:, :, q * 512:(q + 1) * 512])
                        k.dma(POOL, wu_[buf][:], w_eu[l, e].rearrange("(k p) n -> p k n", p=128)[:, :, q * 512:(q + 1) * 512])
                        k.dma(POOL, wd_[buf][:], w_ed[l, e, q * 512:(q + 1) * 512, :].rearrange("(j p) n -> p j n", p=128))

                    load_unit(0, ucnt % 2)
                    for ui, (e, q) in enumerate(units):
                        ub = ucnt % 2
                        ucnt += 1
                        if ui + 1 < len(units):
                            load_unit(ui + 1, ucnt % 2)
                        for sidx, (s0, sn) in enumerate(gsup):
                            W = sn * 128
                            a_ = aT[sidx % 2]
                            for jj in range(4):
                                pg_ = pg[jj % 2]
                                pu_ = pu[jj % 2]
                                for kk in range(8):
                                    k.mm(pg_[:, 0:W], wg_[ub][:, kk, jj * 128:(jj + 1) * 128], hT2[:, kk, s0 * 128:s0 * 128 + W],
                                         start=(kk == 0), stop=(kk == 7))
                                for kk in range(8):
                                    k.mm(pu_[:, 0:W], wu_[ub][:, kk, jj * 128:(jj + 1) * 128], hT2[:, kk, s0 * 128:s0 * 128 + W],
                                         start=(kk == 0), stop=(kk == 7))
                                sg_ = sg[jj % 2]
                                k.act(ACT, sg_[:, 0:W], pg_[:, 0:W], AF.Silu)
                                k.tt(DVE, a_[:, jj, 0:W], sg_[:, 0:W], pu_[:, 0:W], ALU.mult)
                            for i in range(sn):
                                tl = s0 + i
                                ti = grp[tl]
                                for hh in range(2):
                                    pd_ = pd[dcnt % 4]
                                    dcnt += 1
                                    for jj in range(4):
                                        k.mm(pd_[:, :], a_[:, jj, i * 128:(i + 1) * 128], wd_[ub][:, jj, hh * 512:(hh + 1) * 512],
                                             start=(jj == 0), stop=(jj == 3))
                                    k.stt(DVE, yacc[:, tl, hh * 512:(hh + 1) * 512], pd_[:, :], wts[:, ti, e:e + 1],
                                          yacc[:, tl, hh * 512:(hh + 1) * 512], ALU.mult, ALU.add)
                    for tl, ti in enumerate(grp):
                        r0 = ti * 128
                        var = 1 if ti < NTC else 0
                        x_ = xe[tl % 2]
                        k.dma(SP, x_[:], xres[r0:r0 + 128, :])
                        k.tt(POOL, tmpe[:], yacc[:, tl, :], GT2[:, var, :], ALU.mult)
                        k.tt(DVE, x_[:], tmpe[:], x_[:], ALU.add)
                        if not last:
                            k.dma(SP, xres[r0:r0 + 128, :], x_[:])
                        else:
                            rms_rstd(x_[:], junke[:], sse[:], rse[:], D)
                            k.stt(DVE, x_[:], x_[:], rse[:, 0:1], GF[:], ALU.mult, ALU.mult)
                            k.dma(SP, out[(ti - NTC) * 128:(ti - NTC + 1) * 128, :], x_[:])
    return nc


def _consts():
    ident = np.eye(128, dtype=np.float32)
    s = np.arange(128)[:, None]
    t = np.arange(128)[None, :]
    same = (s // 64) == (t // 64)
    triF = (same & (s <= t)).astype(np.float32)
    triFc = (same & (s > t)).astype(np.float32)
    triB = (same & (s >= t)).astype(np.float32)
    triBc = (same & (s < t)).astype(np.float32)
    return ident, np.stack([triF, triFc, triB, triBc])


def make_in_maps(inp, n_cores):
    f = lambda a: np.ascontiguousarray(np.asarray(a, dtype=np.float32))
    L = inp["w_mod"].shape[0]
    ident, masks = _consts()
    shared = {
        "w_mod": f(inp["w_mod"]), "b_mod": f(inp["b_mod"]), "g_norm1": f(inp["g_norm1"]), "g_norm2": f(inp["g_norm2"]),
        "w_in": f(inp["w_in"]), "w_gate2": f(inp["w_gate2"]), "b_gate2": f(inp["b_gate2"]),
        "gla_norm_g": f(inp["gla_norm_g"]),
        "ln_gT": f(np.asarray(inp["sgu_ln_g"]).reshape(L, 8, 128).transpose(0, 2, 1)),
        "sgu_ln_b": f(inp["sgu_ln_b"]),
        "sgu_wT": f(np.asarray(inp["sgu_w"]).transpose(0, 1, 3, 2)),
        "sgu_b": f(inp["sgu_b"]),
        "w_branch_a": f(inp["w_branch_a"]), "w_branch_b": f(inp["w_branch_b"]),
        "b_branchT": f(np.asarray(inp["b_branch"]).reshape(L, 16, 128).transpose(0, 2, 1)),
        "w_out": f(inp["w_out"]), "w_router": f(inp["w_router"]),
        "b_router": f(np.asarray(inp["b_router"]).reshape(1, NE)),
        "w_exp_gate": f(inp["w_exp_gate"]), "w_exp_up": f(inp["w_exp_up"]), "w_exp_down": f(inp["w_exp_down"]),
        "g_final": f(np.asarray(inp["g_final"]).reshape(1, D)),
        "ident": ident, "masks": masks,
    }
    x = np.asarray(inp["x"], dtype=np.float32)
    ctx = np.asarray(inp["ctx"], dtype=np.float32)
    c = np.asarray(inp["c"], dtype=np.float32)
    c_ctx = np.asarray(inp["c_ctx"], dtype=np.float32)
    maps = []
    for b in range(n_cores):
        m = dict(shared)
        m["xin"] = np.ascontiguousarray(np.concatenate([ctx[b], x[b]], axis=0))
        c2 = np.stack([c[b], c_ctx], axis=0)
        m["cT"] = np.ascontiguousarray(c2.reshape(2, 8, 128).transpose(2, 1, 0))[None]
        maps.append(m)
    return maps


_NC_CACHE = {}


def kernel(**inputs):
    x = np.asarray(inputs["x"])
    B, S, _ = x.shape
    C = np.asarray(inputs["ctx"]).shape[1]
    key = (C // 128, S // 128)
    if key not in _NC_CACHE:
        _NC_CACHE[key] = build(NTC=C // 128, NTL=S // 128, L=int(np.asarray(inputs["w_mod"]).shape[0]), NB=1)
    nc = _NC_CACHE[key]
    maps = make_in_maps(inputs, B)
    res = run_bass_kernel_spmd(nc, maps, core_ids=list(range(B)))
    return np.stack([np.asarray(r["out"]).reshape(S, D) for r in res.results], axis=0).astype(np.float32)
```

```python
import contextlib
import numpy as np
import concourse.bass as bass
import concourse.mybir as mybir
from concourse.bass_utils import run_bass_kernel_spmd

F32 = mybir.dt.float32
BF16 = mybir.dt.bfloat16
AF = mybir.ActivationFunctionType
ALU = mybir.AluOpType
AX = mybir.AxisListType

D = 1024
INC = 7200
DE = 1536
NE = 16
EPS = 1e-6
PE, ACT, DVE, POOL, SP = "pe", "act", "dve", "pool", "sp"


class Buf:
    def __init__(self, name):
        self.name = name
        self.last_w = None
        self.readers = []
        self.dsem = None


class DSem:
    def __init__(self, sem):
        self.sem = sem
        self.total = 0


class K:
    def __init__(self, nc, stack):
        self.nc = nc
        self.stack = stack
        self.h = {PE: nc.tensor, ACT: nc.scalar, DVE: nc.vector, POOL: nc.gpsimd, SP: nc.sync}
        self.sem = {}
        self.cnt = {}
        for e in (PE, ACT, DVE, POOL):
            self.sem[e] = stack.enter_context(nc.semaphore("s_" + e))
            self.cnt[e] = 0
        self.seen = {e: {} for e in self.h}
        self.bufs = {}
        self.free_dsems = []
        self.all_dsems = []
        self.n_dsem = 0
        self.phase_bufs = None

    def _reg(self, name, t):
        b = Buf(name)
        self.bufs[name] = b
        if self.phase_bufs is not None:
            self.phase_bufs.append(name)
        return t

    def _uniq(self, name):
        self.n_names = getattr(self, "n_names", 0) + 1
        return "%s_u%d" % (name, self.n_names)

    def sb(self, name, shape, dt, stack=None):
        st = stack or self.cur_stack
        name = self._uniq(name)
        t = st.enter_context(self.nc.sbuf_tensor(name, list(shape), dt))
        return self._reg(name, t)

    def sbu(self, name, shape, dt):
        return self.cur_stack.enter_context(self.nc.sbuf_tensor(self._uniq(name), list(shape), dt))

    def ps(self, name, shape, dt, stack=None):
        st = stack or self.cur_stack
        name = self._uniq(name)
        t = st.enter_context(self.nc.psum_tensor(name, list(shape), dt))
        return self._reg(name, t)

    def get_dsem(self):
        if self.free_dsems:
            return self.free_dsems.pop()
        s = self.stack.enter_context(self.nc.semaphore("d%d" % self.n_dsem))
        self.n_dsem += 1
        ds = DSem(s)
        self.all_dsems.append(ds)
        return ds

    def _wait_tok(self, eng, tok):
        if tok[0] == "e":
            _, src, c = tok
            if src == eng and eng == PE:
                return
            if self.seen[eng].get(src, 0) >= c:
                return
            self.h[eng].wait_ge(self.sem[src], c)
            self.seen[eng][src] = c
        else:
            _, ds, v = tok
            v = max(v, ds.total)
            if self.seen[eng].get(ds, 0) >= v:
                return
            self.h[eng].wait_ge(ds.sem, v)
            self.seen[eng][ds] = v

    def _bufs_of(self, aps):
        out = []
        for a in aps:
            if a is None or isinstance(a, (int, float)):
                continue
            b = self.bufs.get(a.name)
            if b is not None and b not in out:
                out.append(b)
        return out

    def _deps(self, eng, rb, wb):
        for b in rb:
            if b.last_w is not None:
                self._wait_tok(eng, b.last_w)
        for b in wb:
            if b.last_w is not None:
                self._wait_tok(eng, b.last_w)
            for t in b.readers:
                if t[0] == "e" and t[1] == eng:
                    continue
                self._wait_tok(eng, t)

    def I(self, eng, fn, outs, ins):
        rb = self._bufs_of(ins)
        wb = self._bufs_of(outs)
        self._deps(eng, rb, wb)
        inst = fn(self.h[eng])
        self.cnt[eng] += 1
        inst.then_inc(self.sem[eng], 1)
        tok = ("e", eng, self.cnt[eng])
        for b in wb:
            b.last_w = tok
            b.readers = []
        for b in rb:
            if b not in wb:
                b.readers.append(tok)
        return inst

    def dma(self, q, out, in_):
        rb = self._bufs_of([in_])
        wb = self._bufs_of([out])
        self._deps(q, rb, wb)
        sb = (wb or rb)[0]
        if sb.dsem is None:
            sb.dsem = self.get_dsem()
        ds = sb.dsem
        self.h[q].dma_start(out=out, in_=in_).then_inc(ds.sem, 16)
        ds.total += 16
        tok = ("d", ds, ds.total)
        for b in wb:
            b.last_w = tok
            b.readers = []
        for b in rb:
            b.readers.append(tok)

    def barrier(self):
        for e in self.h:
            for src in (PE, ACT, DVE, POOL):
                if src != e and self.cnt[src] > 0:
                    self._wait_tok(e, ("e", src, self.cnt[src]))
            for ds in self.all_dsems:
                if ds.total > 0:
                    self._wait_tok(e, ("d", ds, ds.total))
        for e in (ACT, DVE, POOL):
            if self.cnt[e] > 0 and self.seen[e].get(e, 0) < self.cnt[e]:
                self.h[e].wait_ge(self.sem[e], self.cnt[e])
                self.seen[e][e] = self.cnt[e]

    @contextlib.contextmanager
    def phase(self):
        prev_stack = getattr(self, "cur_stack", None)
        prev_pb = self.phase_bufs
        with contextlib.ExitStack() as st:
            self.cur_stack = st
            self.phase_bufs = []
            yield
            self.barrier()
            for n in self.phase_bufs:
                b = self.bufs.pop(n)
                if b.dsem is not None:
                    self.free_dsems.append(b.dsem)
        self.cur_stack = prev_stack
        self.phase_bufs = prev_pb

    def mm(self, out, lhsT, rhs, start=True, stop=True):
        return self.I(PE, lambda e: e.matmul(out, lhsT, rhs, start=start, stop=stop, skip_group_check=True),
                      [out], [lhsT, rhs])

    def tr(self, out, in_, ident):
        return self.I(PE, lambda e: e.transpose(out, in_, ident), [out], [in_, ident])

    def act(self, eng_unused, out, in_, func, bias=None, scale=None, accum_out=None):
        kw = {}
        if bias is not None:
            kw["bias"] = bias
        if scale is not None:
            kw["scale"] = scale
        if accum_out is not None:
            kw["accum_out"] = accum_out
        return self.I(ACT, lambda e: e.activation(out=out, in_=in_, func=func, **kw), [out, accum_out],
                      [in_, bias, scale])

    def tt(self, eng, out, in0, in1, op):
        return self.I(eng, lambda e: e.tensor_tensor(out=out, in0=in0, in1=in1, op=op), [out], [in0, in1])

    def ts(self, eng, out, in0, s1, s2, op0, op1=None, accum_out=None):
        kw = {}
        if op1 is not None:
            kw["op1"] = op1
        if accum_out is not None:
            kw["accum_out"] = accum_out
        return self.I(eng, lambda e: e.tensor_scalar(out=out, in0=in0, scalar1=s1, scalar2=s2, op0=op0, **kw),
                      [out, accum_out], [in0, s1, s2])

    def stt(self, eng, out, in0, scalar, in1, op0, op1):
        return self.I(eng, lambda e: e.scalar_tensor_tensor(out=out, in0=in0, scalar=scalar, in1=in1, op0=op0, op1=op1),
                      [out], [in0, scalar, in1])

    def cp(self, eng, out, in_):
        if eng == ACT:
            return self.I(ACT, lambda e: e.copy(out=out, in_=in_), [out], [in_])
        return self.I(eng, lambda e: e.tensor_copy(out=out, in_=in_), [out], [in_])

    def memset(self, eng, ap, v):
        return self.I(eng, lambda e: e.memset(ap, v), [ap], [])

    def recip(self, out, in_):
        return self.I(DVE, lambda e: e.reciprocal(out=out, in_=in_), [out], [in_])

    def rmax(self, out, in_):
        return self.I(DVE, lambda e: e.reduce_max(out=out, in_=in_, axis=AX.X), [out], [in_])

    def rsum(self, out, in_):
        return self.I(DVE, lambda e: e.reduce_sum(out=out, in_=in_, axis=AX.X), [out], [in_])


def build(NTC=2, NTL=32, L=2, debug=(), NB=1):
    NT = NTC + NTL
    T = NT * 128
    nc = bass.Bass("TRN2", target_bir_lowering=False)

    def din(name, shape, dt=F32):
        return nc.dram_tensor(name, list(shape), dt, kind="ExternalInput").ap()

    dbg = set(debug)

    def dscr(name, shape, dt):
        kind = "ExternalOutput" if name in dbg else "Internal"
        return nc.dram_tensor(name, list(shape), dt, kind=kind).ap()

    xin_all = din("xin", [NB * T, D])
    cT_all = din("cT", [NB, 128, 8, 2])
    w_mod = din("w_mod", [L, D, 6 * D])
    b_mod = din("b_mod", [L, 6 * D])
    g_n1 = din("g_norm1", [L, D])
    g_n2 = din("g_norm2", [L, D])
    w_in = din("w_in", [L, D, INC])
    w_g2 = din("w_gate2", [L, 2, 16, 512])
    b_g2 = din("b_gate2", [L, 2, 512])
    gla_g = din("gla_norm_g", [L, D])
    ln_gT = din("ln_gT", [L, 128, 8])
    ln_b = din("sgu_ln_b", [L, D])
    sgu_wT = din("sgu_wT", [L, 8, 128, 128])
    sgu_b = din("sgu_b", [L, 8, 128])
    w_a = din("w_branch_a", [L, D, D])
    w_b = din("w_branch_b", [L, D, D])
    bbT = din("b_branchT", [L, 128, 16])
    w_out = din("w_out", [L, D, D])
    w_r = din("w_router", [D, NE])
    b_r = din("b_router", [1, NE])
    w_eg = din("w_exp_gate", [L, NE, D, DE])
    w_eu = din("w_exp_up", [L, NE, D, DE])
    w_ed = din("w_exp_down", [L, NE, DE, D])
    g_fin = din("g_final", [1, D])
    ident_d = din("ident", [128, 128])
    masks_d = din("masks", [4, 128, 128])
    out_all = nc.dram_tensor("out", [NB * NTL * 128, D], F32, kind="ExternalOutput").ap()

    xres = dscr("xres", [T, D], F32)
    modv = dscr("modv", [L, 2, 6 * D], F32)
    uT_d = dscr("uT", [8, 128, T], BF16)
    vg_d = dscr("vg", [T, D], BF16)
    gaT_d = dscr("gaT", [8, 128, T], BF16)
    gbT_d = dscr("gbT", [8, 128, T], BF16)
    yag_d = dscr("yagT", [8, 128, T], BF16)
    v_d = dscr("v_tok", [T, D], BF16)
    sr_d = dscr("sr", [T, D], BF16)
    qin_d = [dscr("qinT%d" % d, [4, 128, T], BF16) for d in range(2)]
    kin_d = [dscr("kinT%d" % d, [4, 128, T], BF16) for d in range(2)]
    kst_d = [dscr("kst%d" % d, [T, 512], BF16) for d in range(2)]
    ob_d = dscr("o_b", [T, D], F32)
    of_d = dscr("o_f", [T, D], F32)
    onT_d = dscr("onT", [8, 128, T], BF16)
    h2T_d = dscr("h2T", [8, 128, T], BF16)

    with contextlib.ExitStack() as top:
        k = K(nc, top)
        k.cur_stack = top
        ident = k.sb("ident", [128, 128], F32)
        identb = k.sb("identb", [128, 128], BF16)
        masks = k.sb("masks_sb", [128, 4, 128], F32)
        decay = k.sb("decay", [128, 2, NT, 8], F32)
        wts = k.sb("wts", [128, NT, NE], F32)
        brt = k.sb("brt", [128, NE], F32)
        wr = k.sb("wr", [128, 8, NE], F32)
        k.dma(SP, ident[:], ident_d[:, :])
        k.dma(SP, masks[:], masks_d.rearrange("m s t -> s m t"))
        k.dma(SP, brt[:], b_r.broadcast_to([128, NE]))
        k.dma(SP, wr[:], w_r.rearrange("(k p) n -> p k n", p=128))
        k.cp(DVE, identb[:], ident[:])
        triF, triFc, triB, triBc = (masks[:, i, :] for i in range(4))

        supers = []
        t0 = 0
        while t0 < NTC:
            n = min(4, NTC - t0)
            supers.append((t0, n))
            t0 += n
        while t0 < NT:
            n = min(4, NT - t0)
            supers.append((t0, n))
            t0 += n

        def rms_rstd(xt_ap, junk_ap, ss, rstd, n):
            k.act(ACT, junk_ap, xt_ap, AF.Square, accum_out=ss)
            k.act(ACT, rstd, ss, AF.Ln, bias=epsb[:, 0:1], scale=1.0 / n)
            k.act(ACT, rstd, rstd, AF.Exp, scale=-0.5)

        epsb = k.sb("epsb", [128, 1], F32)
        k.memset(DVE, epsb[:], EPS)
        oneb = k.sb("oneb", [128, 1], F32)
        k.memset(DVE, oneb[:], 1.0)

        for bl in range(NB * L):
            bi, l = divmod(bl, L)
            xin = xin_all[bi * T:(bi + 1) * T, :]
            cT = cT_all[bi]
            out = out_all[bi * NTL * 128:(bi + 1) * NTL * 128, :]
            last = l == L - 1
            xsrc = xin if l == 0 else xres
            with k.phase():
                cTs = k.sb("cTs", [128, 8, 2], F32)
                scT = k.sb("scT", [128, 8, 2], F32)
                k.dma(SP, cTs[:], cT[:, :, :])
                k.act(ACT, scT[:], cTs[:], AF.Silu)
                modsb = k.sb("modsb", [2, 6 * D], F32)
                bm2 = k.sb("bm2", [2, 6 * D], F32)
                gn = k.sb("gn", [2, 2, D], F32)
                k.dma(SP, bm2[:], b_mod[l:l + 1, :].broadcast_to([2, 6 * D]))
                k.dma(SP, gn[:, 0, :], g_n1[l:l + 1, :].broadcast_to([2, D]))
                k.dma(SP, gn[:, 1, :], g_n2[l:l + 1, :].broadcast_to([2, D]))
                wm = [k.sb("wm%d" % i, [128, 8, 512], F32) for i in range(2)]
                pm = [k.ps("pm%d" % i, [128, 512], F32) for i in range(2)]
                wmv = w_mod[l].rearrange("(k p) n -> p k n", p=128)
                for cc in range(12):
                    w_ = wm[cc % 2]
                    k.dma(SP, w_[:], wmv[:, :, cc * 512:(cc + 1) * 512])
                    p_ = pm[cc % 2]
                    for kk in range(8):
                        k.mm(p_[0:2, :], scT[:, kk, :], w_[:, kk, :], start=(kk == 0), stop=(kk == 7))
                    k.tt(DVE, modsb[:, cc * 512:(cc + 1) * 512], p_[0:2, :], bm2[:, cc * 512:(cc + 1) * 512], ALU.add)
                k.stt(DVE, modsb[:, D:2 * D], modsb[:, D:2 * D], 1.0, gn[:, 0, :], ALU.add, ALU.mult)
                k.stt(DVE, modsb[:, 4 * D:5 * D], modsb[:, 4 * D:5 * D], 1.0, gn[:, 1, :], ALU.add, ALU.mult)
                k.dma(SP, modv[l], modsb[:])

            def bc_load(dst, row, slot):
                k.dma(SP, dst, modv[l, row:row + 1, slot * D:(slot + 1) * D].broadcast_to([128, D]))

            with k.phase():
                wi = k.sb("wi", [128, 8, INC], BF16)
                wiv = w_in[l].rearrange("(k p) n -> p k n", p=128)
                for kk in range(8):
                    k.dma(POOL, wi[:, kk, :], wiv[:, kk, :])
                G1 = k.sb("G1", [128, D], F32)
                SH1 = k.sb("SH1", [128, D], F32)
                cur_var = [-1]
                wg2 = k.sb("wg2", [17, 2, 512], BF16)
                for d in range(2):
                    k.dma(POOL, wg2[0:16, d, :], w_g2[l, d])
                    k.dma(POOL, wg2[16:17, d, :], b_g2[l, d:d + 1, :])
                bb = k.sb("bb", [128, 16], F32)
                k.dma(SP, bb[:], bbT[l])
                xt = [k.sb("xt%d" % i, [128, D], F32) for i in range(2)]
                junk = k.sbu("junk", [128, D], BF16)
                tmp = k.sb("tmpA", [128, D], F32)
                hb = k.sb("hb", [128, D], BF16)
                ss = k.sb("ssA", [128, 1], F32)
                rstd = k.sb("rstdA", [128, 1], F32)
                hT = [k.sb("hT%d" % i, [128, 8, 512], BF16) for i in range(2)]
                pT = k.ps("pT", [128, D], BF16)
                pf = [k.ps("pf%d" % i, [128, 512], F32) for i in range(3)]
                pl = k.ps("pl", [16, 512], F32)
                pt = [k.ps("pt%d" % i, [128, 512], F32) for i in range(3)]
                stg = [k.sb("stg%d" % i, [128, 512], BF16) for i in range(4)]
                scnt = [0]
                qTs = k.sb("qTs", [128, 4, 512], BF16)
                kTs = k.sb("kTs", [128, 4, 512], BF16)
                lr1 = [k.sb("lr1_%d" % d, [32, 512], BF16) for d in range(2)]
                for d in range(2):
                    k.memset(DVE, lr1[d][:], 1.0)
                vgs = k.sb("vgs", [128, D], F32)
                vgb = k.sb("vgb", [128, D], BF16)
                vst = k.sb("vst", [128, 2], F32)
                vsb = k.sb("vsb", [128, D], BF16)
                srb = k.sb("srb", [128, D], BF16)
                ktk = k.sb("ktk", [128, 512], BF16)
                sp_ = k.sb("sp_", [128, 512], F32)
                ebt = k.sb("ebt", [128, 4, 128], F32)
                enbt = k.sb("enbt", [128, 4, 128], F32)
                qinb = k.sb("qinb", [128, 4, 128], BF16)
                kinb = k.sb("kinb", [128, 4, 128], BF16)
                edt = k.sb("edt", [128, 512], F32)
                kstb = k.sb("kstb", [128, 512], BF16)
                fcnt = [0]
                tcnt = [0]

                def nxt_pf():
                    fcnt[0] += 1
                    return pf[fcnt[0] % 3]

                def nxt_pt():
                    tcnt[0] += 1
                    return pt[tcnt[0] % 3]

                def xload(tj):
                    xb_ = xt[tj % 2]
                    k.dma(SP, xb_[:], xsrc[tj * 128:(tj + 1) * 128, :])
                    if l == 0:
                        k.dma(SP, xres[tj * 128:(tj + 1) * 128, :], xb_[:])

                for si, (ts0, nts) in enumerate(supers):
                    W = nts * 128
                    c0 = ts0 * 128
                    var = 1 if ts0 < NTC else 0
                    if cur_var[0] != var:
                        bc_load(G1[:], var, 1)
                        bc_load(SH1[:], var, 0)
                        cur_var[0] = var
                    hTs = hT[si % 2]
                    if si == 0:
                        xload(0)
                    for i in range(nts):
                        ti = ts0 + i
                        x_ = xt[ti % 2]
                        if ti + 1 < NT:
                            xload(ti + 1)
                        rms_rstd(x_[:], junk[:], ss[:], rstd[:], D)
                        k.stt(DVE, tmp[:], x_[:], rstd[:, 0:1], G1[:], ALU.mult, ALU.mult)
                        k.tt(POOL, hb[:], tmp[:], SH1[:], ALU.add)
                        for kk in range(8):
                            k.tr(pT[:, kk * 128:(kk + 1) * 128], hb[:, kk * 128:(kk + 1) * 128], identb[:])
                        k.cp(ACT, hTs[:, :, i * 128:(i + 1) * 128], pT[:].rearrange("p (k t) -> p k t", k=8))

                    def fm(col0, M):
                        p_ = nxt_pf() if M == 128 else pl
                        for kk in range(8):
                            k.mm(p_[0:M, 0:W], wi[:, kk, col0:col0 + M], hTs[:, kk, 0:W], start=(kk == 0), stop=(kk == 7))
                        return p_

                    def nxt_stg():
                        scnt[0] += 1
                        return stg[scnt[0] % 4]

                    for j in range(8):
                        p_ = fm(j * 128, 128)
                        s_ = nxt_stg()
                        k.act(ACT, s_[:, 0:W], p_[:, 0:W], AF.Gelu_apprx_tanh)
                        k.dma(SP, uT_d[j, :, c0:c0 + W], s_[:, 0:W])
                    for h in range(4):
                        p_ = fm(2048 + h * 128, 128)
                        k.act(ACT, qTs[:, h, 0:W], p_[:, 0:W], AF.Copy, scale=128.0 ** -0.5)
                        p_ = fm(2560 + h * 128, 128)
                        k.cp(DVE, kTs[:, h, 0:W], p_[:, 0:W])
                    for d in range(2):
                        p_ = fm(5120 + d * 16, 16)
                        k.cp(DVE, lr1[d][0:16, 0:W], p_[0:16, 0:W])
                    for j in range(16):
                        p_ = fm(5152 + j * 128, 128)
                        dst = gaT_d if j < 8 else gbT_d
                        s_ = nxt_stg()
                        k.act(ACT, s_[:, 0:W], p_[:, 0:W], AF.Sigmoid, bias=bb[:, j:j + 1])
                        k.dma(SP, dst[j % 8, :, c0:c0 + W], s_[:, 0:W])

                    for i in range(nts):
                        ti = ts0 + i
                        r0 = ti * 128
                        tsl = slice(i * 128, (i + 1) * 128)

                        def tm(col0, N):
                            p_ = nxt_pt()
                            for kk in range(8):
                                k.mm(p_[:, 0:N], hTs[:, kk, tsl], wi[:, kk, col0:col0 + N], start=(kk == 0), stop=(kk == 7))
                            return p_

                        for hh in range(2):
                            p_ = tm(1024 + hh * 512, 512)
                            k.act(ACT, vgs[:, hh * 512:(hh + 1) * 512], p_[:, :], AF.Gelu_apprx_tanh)
                        k.rsum(vst[:, 0:1], vgs[:])
                        k.ts(DVE, vst[:, 0:1], vst[:, 0:1], -1.0 / D, None, ALU.mult)
                        k.act(ACT, junk[:], vgs[:], AF.Square, bias=vst[:, 0:1], accum_out=vst[:, 1:2])
                        k.act(ACT, vst[:, 1:2], vst[:, 1:2], AF.Ln, bias=epsb[:, 0:1], scale=1.0 / D)
                        k.act(ACT, vst[:, 1:2], vst[:, 1:2], AF.Exp, scale=-0.5)
                        k.ts(DVE, vgb[:], vgs[:], vst[:, 0:1], vst[:, 1:2], ALU.add, ALU.mult)
                        k.dma(SP, vg_d[r0:r0 + 128, :], vgb[:])
                        p_ = tm(2560, 512)
                        k.cp(ACT, ktk[:], p_[:, :])
                        for hh in range(2):
                            p_ = tm(3072 + hh * 512, 512)
                            k.cp(DVE if hh else ACT, vsb[:, hh * 512:(hh + 1) * 512], p_[:, :])
                        k.dma(SP, v_d[r0:r0 + 128, :], vsb[:])
                        for hh in range(2):
                            p_ = tm(4096 + hh * 512, 512)
                            k.act(ACT, srb[:, hh * 512:(hh + 1) * 512], p_[:, :], AF.Silu)
                        k.dma(SP, sr_d[r0:r0 + 128, :], srb[:])
                        for d in range(2):
                            tri = triF if d == 0 else triB
                            tric = triFc if d == 0 else triBc
                            p_ = nxt_pt()
                            k.mm(p_[:, :], lr1[d][0:17, tsl], wg2[0:17, d, :])
                            k.act(ACT, sp_[:], p_[:, :], AF.Exp, scale=-1.0)
                            k.act(ACT, sp_[:], sp_[:], AF.Ln, bias=oneb[:, 0:1])
                            p2 = nxt_pt()
                            for h in range(4):
                                k.mm(p2[:, h * 128:(h + 1) * 128], sp_[:, h * 128:(h + 1) * 128], tri)
                            p2v = p2[:, :].rearrange("p (h t) -> p h t", h=4)
                            k.act(ACT, ebt[:], p2v, AF.Exp, scale=-1.0 / 16)
                            k.act(ACT, enbt[:], p2v, AF.Exp, scale=1.0 / 16)
                            k.tt(DVE, qinb[:], qTs[:, :, tsl], ebt[:], ALU.mult)
                            k.tt(DVE, kinb[:], kTs[:, :, tsl], enbt[:], ALU.mult)
                            k.dma(SP, qin_d[d][:, :, r0:r0 + 128].rearrange("h p t -> p h t"), qinb[:])
                            k.dma(SP, kin_d[d][:, :, r0:r0 + 128].rearrange("h p t -> p h t"), kinb[:])
                            cs = slice(63, 128, 64) if d == 0 else slice(0, 128, 64)
                            k.cp(DVE, decay[:, d, ti, :].rearrange("p (h c) -> p h c", h=4), ebt[:, :, cs])
                            p3 = nxt_pt()
                            k.mm(p3[:, :], tric, sp_[:])
                            k.act(ACT, edt[:], p3[:, :], AF.Exp, scale=-1.0 / 16)
                            k.tt(DVE, kstb[:], ktk[:], edt[:], ALU.mult)
                            k.dma(SP, kst_d[d][r0:r0 + 128, :], kstb[:])

            with k.phase():
                wa = k.sb("wa", [128, 8, D], BF16)
                k.dma(POOL, wa[:], w_a[l].rearrange("(k p) n -> p k n", p=128))
                wsT = k.sb("wsT", [128, 8, 128], BF16)
                k.dma(POOL, wsT[:], sgu_wT[l].rearrange("g s t -> s g t"))
                wsT32 = k.sb("wsT32", [128, 8, 128], F32)
                k.dma(SP, wsT32[:], sgu_wT[l].rearrange("g s t -> s g t"))
                lng = k.sb("lng", [128, 8], F32)
                k.dma(SP, lng[:], ln_gT[l])
                ones32 = k.sb("ones32", [128, 128], F32)
                k.memset(DVE, ones32[:], 1.0)
                R2 = k.sb("R2", [2, 8, 128], F32)
                L2 = k.sb("L2", [2, D], F32)
                k.memset(DVE, L2[:], 1.0)
                k.dma(SP, L2[0:1, :], ln_b[l:l + 1, :])
                k.dma(SP, R2[1:2, :, :], sgu_b[l:l + 1, :, :])
                pmx = [k.ps("pmx%d" % i, [128, D], F32) for i in range(2)]
                prs, pbs = pmx
                for g in range(8):
                    k.mm(prs[0:1, g * 128:(g + 1) * 128], ones32[:, 0:1], wsT32[:, g, :])
                k.cp(DVE, R2[0:1, :, :], prs[0:1, :].rearrange("p (g t) -> p g t", g=8))
                for g in range(8):
                    k.mm(pbs[:, g * 128:(g + 1) * 128], L2[0:2, g * 128:(g + 1) * 128], R2[0:2, g, :])
                Bias = k.sb("Bias", [128, 8, 128], F32)
                k.cp(DVE, Bias[:], pbs[:, :].rearrange("p (g t) -> p g t", g=8))
                vgt = [k.sb("vgt%d" % i, [128, D], BF16) for i in range(2)]
                uTl = [k.sb("uTl%d" % i, [128, 8, 512], BF16) for i in range(2)]
                gaTl = [k.sb("gaTl%d" % i, [128, 8, 512], BF16) for i in range(2)]
                tmpB = k.sb("tmpB", [128, 8, 128], F32)
                gT = k.sb("gT", [128, 8, 512], BF16)
                py = [k.ps("py%d" % i, [128, 512], F32) for i in range(2)]
                yag = k.sb("yag", [128, 8, 512], BF16)
                def sloadB(sj):
                    tsj, ntj = supers[sj]
                    Wj = ntj * 128
                    cj = tsj * 128
                    k.dma(SP, uTl[sj % 2][:, :, 0:Wj], uT_d[:, :, cj:cj + Wj].rearrange("j p t -> p j t"))
                    k.dma(SP, gaTl[sj % 2][:, :, 0:Wj], gaT_d[:, :, cj:cj + Wj].rearrange("j p t -> p j t"))

                def vloadB(tj):
                    k.dma(SP, vgt[tj % 2][:], vg_d[tj * 128:(tj + 1) * 128, :])

                sloadB(0)
                vloadB(0)
                for si, (ts0, nts) in enumerate(supers):
                    W = nts * 128
                    c0 = ts0 * 128
                    uT_ = uTl[si % 2]
                    ga_ = gaTl[si % 2]
                    if si + 1 < len(supers):
                        sloadB(si + 1)
                    for i in range(nts):
                        ti = ts0 + i
                        v_ = vgt[ti % 2]
                        if ti + 1 < NT:
                            vloadB(ti + 1)
                        pm_ = pmx[ti % 2]
                        for g in range(8):
                            k.mm(pm_[:, g * 128:(g + 1) * 128], v_[:, g * 128:(g + 1) * 128], wsT[:, g, :])
                        pv = pm_[:, :].rearrange("p (g t) -> p g t", g=8)
                        k.tt(DVE, tmpB[:], pv, lng[:, :].unsqueeze(2).broadcast_to([128, 8, 128]), ALU.mult)
                        k.tt(POOL, tmpB[:], tmpB[:], Bias[:], ALU.add)
                        k.tt(DVE, gT[:, :, i * 128:(i + 1) * 128], tmpB[:], uT_[:, :, i * 128:(i + 1) * 128], ALU.mult)
                    for oc in range(8):
                        p_ = py[oc % 2]
                        for kk in range(8):
                            k.mm(p_[:, 0:W], wa[:, kk, oc * 128:(oc + 1) * 128], gT[:, kk, 0:W], start=(kk == 0), stop=(kk == 7))
                        k.tt(DVE, yag[:, oc, 0:W], p_[:, 0:W], ga_[:, oc, 0:W], ALU.mult)
                    k.dma(SP, yag_d[:, :, c0:c0 + W].rearrange("j p t -> p j t"), yag[:, :, 0:W])

            with k.phase():
                zer = k.sb("zer", [128, 128], BF16)
                k.memset(DVE, zer[:], 0.0)
                DS = {}
                for d in (1, 0):
                    st = {}
                    if d == 1:
                        st["order"] = list(range(NTC - 1, -1, -1)) + list(range(NT - 1, NTC - 1, -1))
                        st["co"] = (1, 0)
                        st["msk"] = triB
                    else:
                        st["order"] = list(range(NT))
                        st["co"] = (0, 1)
                        st["msk"] = triF
                    st["S32"] = [k.sb("S32_%d_%d" % (d, h), [128, 256], F32) for h in range(4)]
                    st["S16"] = [k.sb("S16_%d_%d" % (d, h), [128, 256], BF16) for h in range(4)]
                    for h in range(4):
                        k.memset(DVE, st["S32"][h][:], 0.0)
                        k.memset(POOL, st["S16"][h][:], 0.0)
                    st["qn"] = [k.sb("qn%d_%d" % (d, i), [128, 4, 128], BF16) for i in range(2)]
                    st["kn"] = [k.sb("kn%d_%d" % (d, i), [128, 4, 128], BF16) for i in range(2)]
                    st["ks"] = [k.sb("ks%d_%d" % (d, i), [128, 512], BF16) for i in range(2)]
                    st["vv"] = [k.sb("vv%d_%d" % (d, i), [128, D], BF16) for i in range(2)]
                    st["patt"] = k.ps("patt%d" % d, [128, 512], F32)
                    st["po"] = k.ps("po%d" % d, [128, D], F32)
                    st["pkv"] = k.ps("pkv%d" % d, [128, 512], F32)
                    st["att"] = k.sb("att%d" % d, [128, 4, 128], BF16)
                    st["osb"] = [k.sb("osb%d_%d" % (d, i), [128, D], F32) for i in range(2)]
                    st["odst"] = ob_d if d == 1 else of_d
                    DS[d] = st

                def loadsC(d, ii):
                    st = DS[d]
                    ti = st["order"][ii]
                    r0 = ti * 128
                    b_ = ii % 2
                    k.dma(SP, st["qn"][b_][:], qin_d[d][:, :, r0:r0 + 128].rearrange("h p t -> p h t"))
                    k.dma(SP, st["kn"][b_][:], kin_d[d][:, :, r0:r0 + 128].rearrange("h p t -> p h t"))
                    k.dma(SP, st["ks"][b_][:], kst_d[d][r0:r0 + 128, :])
                    k.dma(SP, st["vv"][b_][:], v_d[r0:r0 + 128, :])

                def stage0(d, ii):
                    st = DS[d]
                    b_ = ii % 2
                    q_, k_, v_ = st["qn"][b_], st["kn"][b_], st["vv"][b_]
                    pa, po_, at_ = st["patt"], st["po"], st["att"]
                    for h in range(4):
                        k.mm(pa[:, h * 128:(h + 1) * 128], k_[:, h, :], q_[:, h, :])
                    k.tt(DVE, at_[:], pa[:, :].rearrange("p (h t) -> p h t", h=4),
                         st["msk"].unsqueeze(1).broadcast_to([128, 4, 128]), ALU.mult)
                    for hh in range(2):
                        k.mm(po_[:, hh * 512:(hh + 1) * 512], zer[:], v_[:, hh * 512:(hh + 1) * 512], start=True, stop=False)
                    for h in range(4):
                        k.mm(po_[:, h * 256:(h + 1) * 256], at_[:, h, :], v_[:, h * 256:(h + 1) * 256], start=False, stop=False)

                def stage_chunk(d, ii, ci):
                    st = DS[d]
                    b_ = ii % 2
                    ti = st["order"][ii]
                    c = st["co"][ci]
                    q_, ks_, v_ = st["qn"][b_], st["ks"][b_], st["vv"][b_]
                    po_, pkv, S32, S16 = st["po"], st["pkv"], st["S32"], st["S16"]
                    rs = slice(c * 64, (c + 1) * 64)
                    for h in range(4):
                        k.mm(po_[rs, h * 256:(h + 1) * 256], q_[:, h, rs], S16[h][:], start=False, stop=(ci == 1))
                    for hp in range(2):
                        for h in (2 * hp, 2 * hp + 1):
                            k.mm(pkv[:, (h % 2) * 256:(h % 2 + 1) * 256], ks_[rs, h * 128:(h + 1) * 128], v_[rs, h * 256:(h + 1) * 256])
                        for h in (2 * hp, 2 * hp + 1):
                            k.stt(DVE, S32[h][:], S32[h][:], decay[:, d, ti, h * 2 + c:h * 2 + c + 1],
                                  pkv[:, (h % 2) * 256:(h % 2 + 1) * 256], ALU.mult, ALU.add)
                            k.cp(ACT, S16[h][:], S32[h][:])

                def stage3(d, ii):
                    st = DS[d]
                    b_ = ii % 2
                    ti = st["order"][ii]
                    r0 = ti * 128
                    o_ = st["osb"][b_]
                    po_ = st["po"]
                    k.cp(ACT, o_[:, 0:512], po_[:, 0:512])
                    k.cp(DVE, o_[:, 512:D], po_[:, 512:D])
                    k.dma(SP, st["odst"][r0:r0 + 128, :], o_[:])

                for d in (1, 0):
                    loadsC(d, 0)
                for ii in range(NT):
                    for d in (1, 0):
                        if ii + 1 < NT:
                            loadsC(d, ii + 1)
                    for d in (1, 0):
                        stage0(d, ii)
                    for ci in range(2):
                        for d in (1, 0):
                            stage_chunk(d, ii, ci)
                    for d in (1, 0):
                        stage3(d, ii)

            with k.phase():
                GG = k.sb("GG", [128, D], F32)
                k.dma(SP, GG[:], gla_g[l:l + 1, :].broadcast_to([128, D]))
                ofl = [k.sb("ofl%d" % i, [128, D], F32) for i in range(2)]
                obl = [k.sb("obl%d" % i, [128, D], F32) for i in range(2)]
                srl = [k.sb("srl%d" % i, [128, D], BF16) for i in range(2)]
                osum = [k.sb("osum%d" % i, [128, D], F32) for i in range(2)]
                on32 = [k.sb("on32_%d" % i, [128, D], F32) for i in range(2)]
                onb = [k.sb("onb%d" % i, [128, D], BF16) for i in range(2)]
                junkc = k.sbu("junkc", [128, 256], BF16)
                ssc = [k.sb("ssc%d" % i, [128, 4], F32) for i in range(2)]
                pTc = [k.ps("pTc%d" % i, [128, D], BF16) for i in range(2)]
                onTs = [k.sb("onTs%d" % i, [128, 8, 128], BF16) for i in range(2)]
                tilesC = list(range(NTC, NT)) if last else list(range(NT))

                def loadC2(pos):
                    tj = tilesC[pos]
                    rj = tj * 128
                    k.dma(SP, ofl[pos % 2][:], of_d[rj:rj + 128, :])
                    k.dma(SP, obl[pos % 2][:], ob_d[rj:rj + 128, :])
                    k.dma(SP, srl[pos % 2][:], sr_d[rj:rj + 128, :])

                loadC2(0)
                for pos, ti in enumerate(tilesC):
                    b_ = pos % 2
                    r0 = ti * 128
                    if pos + 1 < len(tilesC):
                        loadC2(pos + 1)
                    o_ = osum[b_]
                    k.tt(DVE, o_[:], ofl[b_][:], obl[b_][:], ALU.add)
                    for h in range(4):
                        k.act(ACT, junkc[:], o_[:, h * 256:(h + 1) * 256], AF.Square, accum_out=ssc[b_][:, h:h + 1])
                    k.act(ACT, ssc[b_][:], ssc[b_][:], AF.Ln, bias=epsb[:, 0:1], scale=1.0 / 256)
                    k.act(ACT, ssc[b_][:], ssc[b_][:], AF.Exp, scale=-0.5)
                    k.tt(DVE, on32[b_][:].rearrange("p (h v) -> p h v", h=4), o_[:].rearrange("p (h v) -> p h v", h=4),
                         ssc[b_][:, :].unsqueeze(2).broadcast_to([128, 4, 256]), ALU.mult)
                    k.tt(DVE, on32[b_][:], on32[b_][:], GG[:], ALU.mult)
                    k.tt(POOL, onb[b_][:], on32[b_][:], srl[b_][:], ALU.mult)
                    for kk in range(8):
                        k.tr(pTc[b_][:, kk * 128:(kk + 1) * 128], onb[b_][:, kk * 128:(kk + 1) * 128], identb[:])
                    k.cp(ACT, onTs[b_][:], pTc[b_][:, :].rearrange("p (k t) -> p k t", k=8))
                    k.dma(SP, onT_d[:, :, r0:r0 + 128].rearrange("j p t -> p j t"), onTs[b_][:])

            with k.phase():
                wb_ = k.sb("wb_", [128, 8, D], BF16)
                wo_ = k.sb("wo_", [128, 8, D], BF16)
                k.dma(POOL, wb_[:], w_b[l].rearrange("(k p) n -> p k n", p=128))
                k.dma(POOL, wo_[:], w_out[l].rearrange("(k p) n -> p k n", p=128))
                GT1 = k.sb("GT1", [128, 2, D], F32)
                G2 = k.sb("G2", [128, 2, D], F32)
                SH2 = k.sb("SH2", [128, 2, D], F32)
                for r in range(2):
                    bc_load(GT1[:, r, :], r, 2)
                    bc_load(G2[:, r, :], r, 4)
                    bc_load(SH2[:, r, :], r, 3)
                onl = [k.sb("onl%d" % i, [128, 8, 512], BF16) for i in range(2)]
                yal = [k.sb("yal%d" % i, [128, 8, 512], BF16) for i in range(2)]
                gbl = [k.sb("gbl%d" % i, [128, 8, 512], BF16) for i in range(2)]
                pyb = [k.ps("pyb%d" % i, [128, 512], F32) for i in range(2)]
                tmpD = k.sb("tmpD", [128, 512], F32)
                mT = k.sb("mT", [128, 8, 512], BF16)
                xl = [k.sb("xl%d" % i, [128, D], F32) for i in range(2)]
                pyo = k.ps("pyo", [128, D], F32)
                tmpx = k.sb("tmpx", [128, D], F32)
                xn = [k.sb("xn%d" % i, [128, D], F32) for i in range(2)]
                junkd = k.sbu("junkd", [128, D], F32)
                ssd = k.sb("ssd", [128, 1], F32)
                rsd = k.sb("rsd", [128, 1], F32)
                h2 = k.sb("h2", [128, D], F32)
                pT32 = k.ps("pT32", [128, D], F32)
                h2T32 = k.sb("h2T32", [128, 8, 128], F32)
                h2Ts = [k.sb("h2Ts%d" % i, [128, 8, 512], BF16) for i in range(2)]
                plg = k.ps("plg", [128, NE], F32)
                lg4 = k.sb("lg4", [128, 4, NE], F32)
                pr4 = k.sb("pr4", [128, 4, NE], F32)
                p24 = k.sb("p24", [128, 4, NE], F32)
                eq4 = k.sb("eq4", [128, 4, NE], F32)
                sel4 = k.sb("sel4", [128, 4, NE], F32)
                r4 = k.sb("r4", [128, 4, 4], F32)
                m14 = k.sb("m14", [128, 16], F32)
                m24 = k.sb("m24", [128, 16], F32)
                gs4 = k.sb("gs4", [128, 16], F32)
                gmk4 = k.sb("gmk4", [128, 16], F32)
                lg = k.sb("lg", [128, NE], F32)
                rt = k.sb("rt", [128, 8], F32)
                pr = k.sb("pr", [128, NE], F32)
                p2_ = k.sb("p2_", [128, NE], F32)
                eq = k.sb("eq", [128, NE], F32)
                m1 = k.sb("m1", [128, 4], F32)
                m2 = k.sb("m2", [128, 4], F32)
                gs = k.sb("gs", [128, 4], F32)
                gmk = k.sb("gmk", [128, 4], F32)
                sel = k.sb("sel", [128, NE], F32)

                def g4(ap):
                    return ap.rearrange("p (g e) -> p g e", g=4)

                def b4(ap):
                    return ap.unsqueeze(2).broadcast_to([128, 4, 4])

                supD = [sp for sp in supers if not (last and sp[0] < NTC)]
                tilesD = [ts_ + i_ for (ts_, n_) in supD for i_ in range(n_)]

                def sloadD(sj):
                    tsj, ntj = supD[sj]
                    Wj = ntj * 128
                    cj = tsj * 128
                    k.dma(SP, onl[sj % 2][:, :, 0:Wj], onT_d[:, :, cj:cj + Wj].rearrange("j p t -> p j t"))
                    k.dma(SP, yal[sj % 2][:, :, 0:Wj], yag_d[:, :, cj:cj + Wj].rearrange("j p t -> p j t"))
                    k.dma(SP, gbl[sj % 2][:, :, 0:Wj], gbT_d[:, :, cj:cj + Wj].rearrange("j p t -> p j t"))

                def xloadD(tpos):
                    tj = tilesD[tpos]
                    k.dma(SP, xl[tpos % 2][:], xres[tj * 128:(tj + 1) * 128, :])

                sloadD(0)
                xloadD(0)
                tposD = 0
                for si, (ts0, nts) in enumerate(supD):
                    W = nts * 128
                    c0 = ts0 * 128
                    var = 1 if ts0 < NTC else 0
                    on_, ya_, gb_ = onl[si % 2], yal[si % 2], gbl[si % 2]
                    if si + 1 < len(supD):
                        sloadD(si + 1)
                    for oc in range(8):
                        p_ = pyb[oc % 2]
                        for kk in range(8):
                            k.mm(p_[:, 0:W], wb_[:, kk, oc * 128:(oc + 1) * 128], on_[:, kk, 0:W], start=(kk == 0), stop=(kk == 7))
                        k.tt(DVE, tmpD[:, 0:W], p_[:, 0:W], gb_[:, oc, 0:W], ALU.mult)
                        k.tt(POOL, mT[:, oc, 0:W], tmpD[:, 0:W], ya_[:, oc, 0:W], ALU.add)
                    h2T_ = h2Ts[si % 2]
                    for i in range(nts):
                        ti = ts0 + i
                        r0 = ti * 128
                        x_ = xl[tposD % 2]
                        xn_ = xn[tposD % 2]
                        tposD += 1
                        if tposD < len(tilesD):
                            xloadD(tposD)
                        for hh in range(2):
                            for kk in range(8):
                                k.mm(pyo[:, hh * 512:(hh + 1) * 512], mT[:, kk, i * 128:(i + 1) * 128],
                                     wo_[:, kk, hh * 512:(hh + 1) * 512], start=(kk == 0), stop=(kk == 7))
                        k.tt(DVE, tmpx[:], pyo[:, :], GT1[:, var, :], ALU.mult)
                        k.tt(POOL, xn_[:], tmpx[:], x_[:], ALU.add)
                        k.dma(SP, xres[r0:r0 + 128, :], xn_[:])
                        rms_rstd(xn_[:], junkd[:], ssd[:], rsd[:], D)
                        k.stt(DVE, tmpx[:], xn_[:], rsd[:, 0:1], G2[:, var, :], ALU.mult, ALU.mult)
                        k.tt(POOL, h2[:], tmpx[:], SH2[:, var, :], ALU.add)
                        for kk in range(8):
                            k.tr(pT32[:, kk * 128:(kk + 1) * 128], h2[:, kk * 128:(kk + 1) * 128], ident[:])
                        k.cp(ACT, h2T32[:], pT32[:, :].rearrange("p (k t) -> p k t", k=8))
                        k.cp(DVE, h2T_[:, :, i * 128:(i + 1) * 128], h2T32[:])
                        for kk in range(8):
                            k.mm(plg[:, :], h2T32[:, kk, :], wr[:, kk, :], start=(kk == 0), stop=(kk == 7))
                        k.tt(DVE, lg4[:, i, :], plg[:, :], brt[:], ALU.add)
                    n_ = nts
                    L3 = lg4[:, 0:n_, :]
                    P3 = pr4[:, 0:n_, :]
                    Q3 = p24[:, 0:n_, :]
                    E3 = eq4[:, 0:n_, :]
                    S3 = sel4[:, 0:n_, :]

                    def ge(ap):
                        return ap.rearrange("p j (g e) -> p (j g) e", g=4)

                    def bl(ap, m):
                        return ap.unsqueeze(2).broadcast_to([128, ap.shape[1], m])

                    k.I(DVE, lambda e: e.tensor_reduce(out=r4[:, 0, 0:n_], in_=L3, axis=AX.X, op=ALU.max), [r4[:]], [lg4[:]])
                    k.tt(DVE, L3, L3, bl(r4[:, 0, 0:n_], NE), ALU.subtract)
                    k.act(ACT, P3, L3, AF.Exp)
                    k.I(DVE, lambda e: e.tensor_reduce(out=r4[:, 1, 0:n_], in_=P3, axis=AX.X, op=ALU.add), [r4[:]], [pr4[:]])
                    k.recip(r4[:, 1, 0:n_], r4[:, 1, 0:n_])
                    k.tt(DVE, P3, P3, bl(r4[:, 1, 0:n_], NE), ALU.mult)
                    k.I(DVE, lambda e: e.tensor_reduce(out=m14[:, 0:4 * n_], in_=ge(P3), axis=AX.X, op=ALU.max), [m14[:]], [pr4[:]])
                    k.tt(DVE, ge(E3), ge(P3), bl(m14[:, 0:4 * n_], 4), ALU.is_ge)
                    k.stt(DVE, Q3, E3, -4.0, P3, ALU.mult, ALU.add)
                    k.I(DVE, lambda e: e.tensor_reduce(out=m24[:, 0:4 * n_], in_=ge(Q3), axis=AX.X, op=ALU.max), [m24[:]], [p24[:]])
                    k.tt(DVE, gs4[:, 0:4 * n_], m14[:, 0:4 * n_], m24[:, 0:4 * n_], ALU.add)
                    gsv = gs4[:, 0:4 * n_].rearrange("p (j g) -> p j g", g=4)
                    k.I(DVE, lambda e: e.tensor_reduce(out=r4[:, 2, 0:n_], in_=gsv, axis=AX.X, op=ALU.max), [r4[:]], [gs4[:]])
                    k.tt(DVE, gmk4[:, 0:4 * n_].rearrange("p (j g) -> p j g", g=4), gsv, bl(r4[:, 2, 0:n_], 4), ALU.is_ge)
                    k.tt(DVE, ge(S3), ge(P3), bl(m24[:, 0:4 * n_], 4), ALU.is_ge)
                    k.tt(DVE, ge(S3), ge(S3), bl(gmk4[:, 0:4 * n_], 4), ALU.mult)
                    k.tt(DVE, S3, S3, P3, ALU.mult)
                    k.I(DVE, lambda e: e.tensor_reduce(out=r4[:, 3, 0:n_], in_=S3, axis=AX.X, op=ALU.add), [r4[:]], [sel4[:]])
                    k.recip(r4[:, 3, 0:n_], r4[:, 3, 0:n_])
                    k.tt(DVE, wts[:, ts0:ts0 + n_, :], S3, bl(r4[:, 3, 0:n_], NE), ALU.mult)
                    k.dma(SP, h2T_d[:, :, c0:c0 + W].rearrange("j p t -> p j t"), h2T_[:, :, 0:W])

            moe_tiles = list(range(NTC, NT)) if last else list(range(NT))
            GSZ = 17
            groups = [moe_tiles[i:i + GSZ] for i in range(0, len(moe_tiles), GSZ)]
            with k.phase():
                GT2 = k.sb("GT2", [128, 2, D], F32)
                for r in range(2):
                    bc_load(GT2[:, r, :], r, 5)
                if last:
                    GF = k.sb("GF", [128, D], F32)
                    k.dma(SP, GF[:], g_fin.broadcast_to([128, D]))
                hT2 = k.sb("hT2", [128, 8, GSZ * 128], BF16)
                yacc = k.sb("yacc", [128, GSZ, D], F32)
                wg_ = [k.sb("wg_%d" % i, [128, 8, 512], BF16) for i in range(2)]
                wu_ = [k.sb("wu_%d" % i, [128, 8, 512], BF16) for i in range(2)]
                wd_ = [k.sb("wd_%d" % i, [128, 4, D], BF16) for i in range(2)]
                pg = [k.ps("pg%d" % i, [128, 512], F32) for i in range(2)]
                pu = [k.ps("pu%d" % i, [128, 512], F32) for i in range(2)]
                pd = [k.ps("pd%d" % i, [128, 512], F32) for i in range(4)]
                sg = [k.sb("sg%d" % i, [128, 512], F32) for i in range(2)]
                aT = [k.sb("aT%d" % i, [128, 4, 512], BF16) for i in range(2)]
                xe = [k.sb("xe%d" % i, [128, D], F32) for i in range(2)]
                tmpe = k.sb("tmpe", [128, D], F32)
                junke = k.sbu("junke", [128, D], F32)
                sse = k.sb("sse", [128, 1], F32)
                rse = k.sb("rse", [128, 1], F32)
                ucnt = 0
                dcnt = 0
                for grp in groups:
                    ng = len(grp)
                    g0 = grp[0] * 128
                    WG = ng * 128
                    k.dma(SP, hT2[:, :, 0:WG], h2T_d[:, :, g0:g0 + WG].rearrange("j p t -> p j t"))
                    k.memset(POOL, yacc[:, 0:ng, :], 0.0)
                    gsup = [(s, min(4, ng - s)) for s in range(0, ng, 4)]
                    units = [(e, q) for e in range(NE) for q in range(3)]

                    def load_unit(ui, buf):
                        e, q = units[ui]
                        k.dma(POOL, wg_[buf][:], w_eg[l, e].rearrange("(k p) n -> p k n", p=128)[:, :, q * 512:(q + 1) * 512])
                        k.dma(POOL, wu_[buf][:], w_eu[l, e].rearrange("(k p) n -> p k n", p=128)[:, :, q * 512:(q + 1) * 512])
                        k.dma(POOL, wd_[buf][:], w_ed[l, e, q * 512:(q + 1) * 512, :].rearrange("(j p) n -> p j n", p=128))

                    load_unit(0, ucnt % 2)
                    for ui, (e, q) in enumerate(units):
                        ub = ucnt % 2
                        ucnt += 1
                        if ui + 1 < len(units):
                            load_unit(ui + 1, ucnt % 2)
                        for sidx, (s0, sn) in enumerate(gsup):
                            W = sn * 128
                            a_ = aT[sidx % 2]
                            for jj in range(4):
                                pg_ = pg[jj % 2]
                                pu_ = pu[jj % 2]
                                for kk in range(8):
                                    k.mm(pg_[:, 0:W], wg_[ub][:, kk, jj * 128:(jj + 1) * 128], hT2[:, kk, s0 * 128:s0 * 128 + W],
                                         start=(kk == 0), stop=(kk == 7))
                                for kk in range(8):
                                    k.mm(pu_[:, 0:W], wu_[ub][:, kk, jj * 128:(jj + 1) * 128], hT2[:, kk, s0 * 128:s0 * 128 + W],
                                         start=(kk == 0), stop=(kk == 7))
                                sg_ = sg[jj % 2]
                                k.act(ACT, sg_[:, 0:W], pg_[:, 0:W], AF.Silu)
                                k.tt(DVE, a_[:, jj, 0:W], sg_[:, 0:W], pu_[:, 0:W], ALU.mult)
                            for i in range(sn):
                                tl = s0 + i
                                ti = grp[tl]
                                for hh in range(2):
                                    pd_ = pd[dcnt % 4]
                                    dcnt += 1
                                    for jj in range(4):
                                        k.mm(pd_[:, :], a_[:, jj, i * 128:(i + 1) * 128], wd_[ub][:, jj, hh * 512:(hh + 1) * 512],
                                             start=(jj == 0), stop=(jj == 3))
                                    k.stt(DVE, yacc[:, tl, hh * 512:(hh + 1) * 512], pd_[:, :], wts[:, ti, e:e + 1],
                                          yacc[:, tl, hh * 512:(hh + 1) * 512], ALU.mult, ALU.add)
                    for tl, ti in enumerate(grp):
                        r0 = ti * 128
                        var = 1 if ti < NTC else 0
                        x_ = xe[tl % 2]
                        k.dma(SP, x_[:], xres[r0:r0 + 128, :])
                        k.tt(POOL, tmpe[:], yacc[:, tl, :], GT2[:, var, :], ALU.mult)
                        k.tt(DVE, x_[:], tmpe[:], x_[:], ALU.add)
                        if not last:
                            k.dma(SP, xres[r0:r0 + 128, :], x_[:])
                        else:
                            rms_rstd(x_[:], junke[:], sse[:], rse[:], D)
                            k.stt(DVE, x_[:], x_[:], rse[:, 0:1], GF[:], ALU.mult, ALU.mult)
                            k.dma(SP, out[(ti - NTC) * 128:(ti - NTC + 1) * 128, :], x_[:])
    return nc


def _consts():
    ident = np.eye(128, dtype=np.float32)
    s = np.arange(128)[:, None]
    t = np.arange(128)[None, :]
    same = (s // 64) == (t // 64)
    triF = (same & (s <= t)).astype(np.float32)
    triFc = (same & (s > t)).astype(np.float32)
    triB = (same & (s >= t)).astype(np.float32)
    triBc = (same & (s < t)).astype(np.float32)
    return ident, np.stack([triF, triFc, triB, triBc])


def make_in_maps(inp, n_cores):
    f = lambda a: np.ascontiguousarray(np.asarray(a, dtype=np.float32))
    L = inp["w_mod"].shape[0]
    ident, masks = _consts()
    shared = {
        "w_mod": f(inp["w_mod"]), "b_mod": f(inp["b_mod"]), "g_norm1": f(inp["g_norm1"]), "g_norm2": f(inp["g_norm2"]),
        "w_in": f(inp["w_in"]), "w_gate2": f(inp["w_gate2"]), "b_gate2": f(inp["b_gate2"]),
        "gla_norm_g": f(inp["gla_norm_g"]),
        "ln_gT": f(np.asarray(inp["sgu_ln_g"]).reshape(L, 8, 128).transpose(0, 2, 1)),
        "sgu_ln_b": f(inp["sgu_ln_b"]),
        "sgu_wT": f(np.asarray(inp["sgu_w"]).transpose(0, 1, 3, 2)),
        "sgu_b": f(inp["sgu_b"]),
        "w_branch_a": f(inp["w_branch_a"]), "w_branch_b": f(inp["w_branch_b"]),
        "b_branchT": f(np.asarray(inp["b_branch"]).reshape(L, 16, 128).transpose(0, 2, 1)),
        "w_out": f(inp["w_out"]), "w_router": f(inp["w_router"]),
        "b_router": f(np.asarray(inp["b_router"]).reshape(1, NE)),
        "w_exp_gate": f(inp["w_exp_gate"]), "w_exp_up": f(inp["w_exp_up"]), "w_exp_down": f(inp["w_exp_down"]),
        "g_final": f(np.asarray(inp["g_final"]).reshape(1, D)),
        "ident": ident, "masks": masks,
    }
    x = np.asarray(inp["x"], dtype=np.float32)
    ctx = np.asarray(inp["ctx"], dtype=np.float32)
    c = np.asarray(inp["c"], dtype=np.float32)
    c_ctx = np.asarray(inp["c_ctx"], dtype=np.float32)
    maps = []
    for b in range(n_cores):
        m = dict(shared)
        m["xin"] = np.ascontiguousarray(np.concatenate([ctx[b], x[b]], axis=0))
        c2 = np.stack([c[b], c_ctx], axis=0)
        m["cT"] = np.ascontiguousarray(c2.reshape(2, 8, 128).transpose(2, 1, 0))[None]
        maps.append(m)
    return maps


_NC_CACHE = {}


def kernel(**inputs):
    x = np.asarray(inputs["x"])
    B, S, _ = x.shape
    C = np.asarray(inputs["ctx"]).shape[1]
    key = (C // 128, S // 128)
    if key not in _NC_CACHE:
        _NC_CACHE[key] = build(NTC=C // 128, NTL=S // 128, L=int(np.asarray(inputs["w_mod"]).shape[0]), NB=1)
    nc = _NC_CACHE[key]
    maps = make_in_maps(inputs, B)
    res = run_bass_kernel_spmd(nc, maps, core_ids=list(range(B)))
    return np.stack([np.asarray(r["out"]).reshape(S, D) for r in res.results], axis=0).astype(np.float32)
```

```python
import contextlib
import numpy as np
import concourse.bass as bass
import concourse.mybir as mybir
from concourse.bass_utils import run_bass_kernel_spmd

F32 = mybir.dt.float32
BF16 = mybir.dt.bfloat16
AF = mybir.ActivationFunctionType
ALU = mybir.AluOpType
AX = mybir.AxisListType

D = 1024
INC = 7200
DE = 1536
NE = 16
EPS = 1e-6
PE, ACT, DVE, POOL, SP = "pe", "act", "dve", "pool", "sp"


class Buf:
    def __init__(self, name):
        self.name = name
        self.last_w = None
        self.readers = []
        self.dsem = None


class DSem:
    def __init__(self, sem):
        self.sem = sem
        self.total = 0


class K:
    def __init__(self, nc, stack):
        self.nc = nc
        self.stack = stack
        self.h = {PE: nc.tensor, ACT: nc.scalar, DVE: nc.vector, POOL: nc.gpsimd, SP: nc.sync}
        self.sem = {}
        self.cnt = {}
        for e in (PE, ACT, DVE, POOL):
            self.sem[e] = stack.enter_context(nc.semaphore("s_" + e))
            self.cnt[e] = 0
        self.seen = {e: {} for e in self.h}
        self.bufs = {}
        self.free_dsems = {}
        self.all_dsems = []
        self.n_dsem = 0
        self.phase_bufs = None

    def _reg(self, name, t):
        b = Buf(name)
        self.bufs[name] = b
        if self.phase_bufs is not None:
            self.phase_bufs.append(name)
        return t

    def _uniq(self, name):
        self.n_names = getattr(self, "n_names", 0) + 1
        return "%s_u%d" % (name, self.n_names)

    def sb(self, name, shape, dt, stack=None):
        st = stack or self.cur_stack
        name = self._uniq(name)
        t = st.enter_context(self.nc.sbuf_tensor(name, list(shape), dt))
        return self._reg(name, t)

    def sbu(self, name, shape, dt):
        return self.cur_stack.enter_context(self.nc.sbuf_tensor(self._uniq(name), list(shape), dt))

    def ps(self, name, shape, dt, stack=None):
        st = stack or self.cur_stack
        name = self._uniq(name)
        t = st.enter_context(self.nc.psum_tensor(name, list(shape), dt))
        return self._reg(name, t)

    def get_dsem(self, q):
        pool = self.free_dsems.setdefault(q, [])
        if pool:
            return pool.pop()
        s = self.stack.enter_context(self.nc.semaphore("d%d" % self.n_dsem))
        self.n_dsem += 1
        ds = DSem(s)
        ds.q = q
        self.all_dsems.append(ds)
        return ds

    def _wait_tok(self, eng, tok):
        if tok[0] == "e":
            _, src, c = tok
            if src == eng and eng == PE:
                return
            if self.seen[eng].get(src, 0) >= c:
                return
            self.h[eng].wait_ge(self.sem[src], c)
            self.seen[eng][src] = c
        else:
            _, ds, v = tok
            v = max(v, ds.total)
            if self.seen[eng].get(ds, 0) >= v:
                return
            self.h[eng].wait_ge(ds.sem, v)
            self.seen[eng][ds] = v

    def _bufs_of(self, aps):
        out = []
        for a in aps:
            if a is None or isinstance(a, (int, float)):
                continue
            b = self.bufs.get(a.name)
            if b is not None and b not in out:
                out.append(b)
        return out

    def _deps(self, eng, rb, wb):
        for b in rb:
            if b.last_w is not None:
                self._wait_tok(eng, b.last_w)
        for b in wb:
            if b.last_w is not None:
                self._wait_tok(eng, b.last_w)
            for t in b.readers:
                if t[0] == "e" and t[1] == eng:
                    continue
                self._wait_tok(eng, t)

    def I(self, eng, fn, outs, ins):
        rb = self._bufs_of(ins)
        wb = self._bufs_of(outs)
        self._deps(eng, rb, wb)
        inst = fn(self.h[eng])
        self.cnt[eng] += 1
        inst.then_inc(self.sem[eng], 1)
        tok = ("e", eng, self.cnt[eng])
        for b in wb:
            b.last_w = tok
            b.readers = []
        for b in rb:
            if b not in wb:
                b.readers.append(tok)
        return inst

    def dma(self, q, out, in_):
        rb = self._bufs_of([in_])
        wb = self._bufs_of([out])
        self._deps(q, rb, wb)
        sb = (wb or rb)[0]
        if sb.dsem is None:
            sb.dsem = {}
        if q not in sb.dsem:
            sb.dsem[q] = self.get_dsem(q)
        ds = sb.dsem[q]
        self.h[q].dma_start(out=out, in_=in_).then_inc(ds.sem, 16)
        ds.total += 16
        tok = ("d", ds, ds.total)
        for b in wb:
            b.last_w = tok
            b.readers = []
        for b in rb:
            b.readers.append(tok)

    def barrier(self):
        for e in self.h:
            for src in (PE, ACT, DVE, POOL):
                if src != e and self.cnt[src] > 0:
                    self._wait_tok(e, ("e", src, self.cnt[src]))
            for ds in self.all_dsems:
                if ds.total > 0:
                    self._wait_tok(e, ("d", ds, ds.total))
        for e in (ACT, DVE, POOL):
            if self.cnt[e] > 0 and self.seen[e].get(e, 0) < self.cnt[e]:
                self.h[e].wait_ge(self.sem[e], self.cnt[e])
                self.seen[e][e] = self.cnt[e]

    @contextlib.contextmanager
    def phase(self):
        prev_stack = getattr(self, "cur_stack", None)
        prev_pb = self.phase_bufs
        with contextlib.ExitStack() as st:
            self.cur_stack = st
            self.phase_bufs = []
            yield
            self.barrier()
            for n in self.phase_bufs:
                b = self.bufs.pop(n)
                if b.dsem is not None:
                    for q_, ds_ in b.dsem.items():
                        self.free_dsems.setdefault(q_, []).append(ds_)
        self.cur_stack = prev_stack
        self.phase_bufs = prev_pb

    def mm(self, out, lhsT, rhs, start=True, stop=True):
        return self.I(PE, lambda e: e.matmul(out, lhsT, rhs, start=start, stop=stop, skip_group_check=True),
                      [out], [lhsT, rhs])

    def tr(self, out, in_, ident):
        return self.I(PE, lambda e: e.transpose(out, in_, ident), [out], [in_, ident])

    def act(self, eng_unused, out, in_, func, bias=None, scale=None, accum_out=None):
        kw = {}
        if bias is not None:
            kw["bias"] = bias
        if scale is not None:
            kw["scale"] = scale
        if accum_out is not None:
            kw["accum_out"] = accum_out
        return self.I(ACT, lambda e: e.activation(out=out, in_=in_, func=func, **kw), [out, accum_out],
                      [in_, bias, scale])

    def tt(self, eng, out, in0, in1, op):
        return self.I(eng, lambda e: e.tensor_tensor(out=out, in0=in0, in1=in1, op=op), [out], [in0, in1])

    def ts(self, eng, out, in0, s1, s2, op0, op1=None, accum_out=None):
        kw = {}
        if op1 is not None:
            kw["op1"] = op1
        if accum_out is not None:
            kw["accum_out"] = accum_out
        return self.I(eng, lambda e: e.tensor_scalar(out=out, in0=in0, scalar1=s1, scalar2=s2, op0=op0, **kw),
                      [out, accum_out], [in0, s1, s2])

    def stt(self, eng, out, in0, scalar, in1, op0, op1):
        return self.I(eng, lambda e: e.scalar_tensor_tensor(out=out, in0=in0, scalar=scalar, in1=in1, op0=op0, op1=op1),
                      [out], [in0, scalar, in1])

    def cp(self, eng, out, in_):
        if eng == ACT:
            return self.I(ACT, lambda e: e.copy(out=out, in_=in_), [out], [in_])
        return self.I(eng, lambda e: e.tensor_copy(out=out, in_=in_), [out], [in_])

    def memset(self, eng, ap, v):
        return self.I(eng, lambda e: e.memset(ap, v), [ap], [])

    def recip(self, out, in_):
        return self.I(DVE, lambda e: e.reciprocal(out=out, in_=in_), [out], [in_])

    def rmax(self, out, in_):
        return self.I(DVE, lambda e: e.reduce_max(out=out, in_=in_, axis=AX.X), [out], [in_])

    def rsum(self, out, in_):
        return self.I(DVE, lambda e: e.reduce_sum(out=out, in_=in_, axis=AX.X), [out], [in_])


def build(NTC=2, NTL=32, L=2, debug=(), NB=1):
    NT = NTC + NTL
    T = NT * 128
    nc = bass.Bass("TRN2", target_bir_lowering=False)

    def din(name, shape, dt=F32):
        return nc.dram_tensor(name, list(shape), dt, kind="ExternalInput").ap()

    dbg = set(debug)

    def dscr(name, shape, dt):
        kind = "ExternalOutput" if name in dbg else "Internal"
        return nc.dram_tensor(name, list(shape), dt, kind=kind).ap()

    xin_all = din("xin", [NB * T, D])
    cT_all = din("cT", [NB, 128, 8, 2])
    w_mod = din("w_mod", [L, D, 6 * D])
    b_mod = din("b_mod", [L, 6 * D])
    g_n1 = din("g_norm1", [L, D])
    g_n2 = din("g_norm2", [L, D])
    w_in = din("w_in", [L, D, INC])
    w_g2 = din("w_gate2", [L, 2, 16, 512])
    b_g2 = din("b_gate2", [L, 2, 512])
    gla_g = din("gla_norm_g", [L, D])
    ln_gT = din("ln_gT", [L, 128, 8])
    ln_b = din("sgu_ln_b", [L, D])
    sgu_wT = din("sgu_wT", [L, 8, 128, 128])
    sgu_b = din("sgu_b", [L, 8, 128])
    w_a = din("w_branch_a", [L, D, D])
    w_b = din("w_branch_b", [L, D, D])
    bbT = din("b_branchT", [L, 128, 16])
    w_out = din("w_out", [L, D, D])
    w_r = din("w_router", [D, NE])
    b_r = din("b_router", [1, NE])
    w_eg = din("w_exp_gate", [L, NE, D, DE])
    w_eu = din("w_exp_up", [L, NE, D, DE])
    w_ed = din("w_exp_down", [L, NE, DE, D])
    g_fin = din("g_final", [1, D])
    ident_d = din("ident", [128, 128])
    masks_d = din("masks", [4, 128, 128])
    out_all = nc.dram_tensor("out", [NB * NTL * 128, D], F32, kind="ExternalOutput").ap()

    xres = dscr("xres", [T, D], F32)
    modv = dscr("modv", [L, 2, 6 * D], F32)
    uT_d = dscr("uT", [8, 128, T], BF16)
    vg_d = dscr("vg", [T, D], BF16)
    gaT_d = dscr("gaT", [8, 128, T], BF16)
    gbT_d = dscr("gbT", [8, 128, T], BF16)
    yag_d = dscr("yagT", [8, 128, T], BF16)
    v_d = dscr("v_tok", [T, D], BF16)
    sr_d = dscr("sr", [T, D], BF16)
    qin_d = [dscr("qinT%d" % d, [4, 128, T], BF16) for d in range(2)]
    kin_d = [dscr("kinT%d" % d, [4, 128, T], BF16) for d in range(2)]
    kst_d = [dscr("kst%d" % d, [T, 512], BF16) for d in range(2)]
    ob_d = dscr("o_b", [T, D], F32)
    of_d = dscr("o_f", [T, D], F32)
    onT_d = dscr("onT", [8, 128, T], BF16)
    h2T_d = dscr("h2T", [8, 128, T], BF16)

    with contextlib.ExitStack() as top:
        k = K(nc, top)
        k.cur_stack = top
        ident = k.sb("ident", [128, 128], F32)
        identb = k.sb("identb", [128, 128], BF16)
        masks = k.sb("masks_sb", [128, 4, 128], F32)
        decay = k.sb("decay", [128, 2, NT, 8], F32)
        wts = k.sb("wts", [128, NT, NE], F32)
        brt = k.sb("brt", [128, NE], F32)
        wr = k.sb("wr", [128, 8, NE], F32)
        k.dma(SP, ident[:], ident_d[:, :])
        k.dma(SP, masks[:], masks_d.rearrange("m s t -> s m t"))
        k.dma(SP, brt[:], b_r.broadcast_to([128, NE]))
        k.dma(SP, wr[:], w_r.rearrange("(k p) n -> p k n", p=128))
        k.cp(DVE, identb[:], ident[:])
        triF, triFc, triB, triBc = (masks[:, i, :] for i in range(4))

        supers = []
        t0 = 0
        while t0 < NTC:
            n = min(4, NTC - t0)
            supers.append((t0, n))
            t0 += n
        while t0 < NT:
            n = min(4, NT - t0)
            supers.append((t0, n))
            t0 += n

        def rms_rstd(xt_ap, junk_ap, ss, rstd, n):
            k.act(ACT, junk_ap, xt_ap, AF.Square, accum_out=ss)
            k.act(ACT, rstd, ss, AF.Ln, bias=epsb[:, 0:1], scale=1.0 / n)
            k.act(ACT, rstd, rstd, AF.Exp, scale=-0.5)

        epsb = k.sb("epsb", [128, 1], F32)
        k.memset(DVE, epsb[:], EPS)
        oneb = k.sb("oneb", [128, 1], F32)
        k.memset(DVE, oneb[:], 1.0)

        for bl in range(NB * L):
            bi, l = divmod(bl, L)
            xin = xin_all[bi * T:(bi + 1) * T, :]
            cT = cT_all[bi]
            out = out_all[bi * NTL * 128:(bi + 1) * NTL * 128, :]
            last = l == L - 1
            xsrc = xin if l == 0 else xres
            with k.phase():
                cTs = k.sb("cTs", [128, 8, 2], F32)
                scT = k.sb("scT", [128, 8, 2], F32)
                k.dma(SP, cTs[:], cT[:, :, :])
                k.act(ACT, scT[:], cTs[:], AF.Silu)
                modsb = k.sb("modsb", [2, 6 * D], F32)
                bm2 = k.sb("bm2", [2, 6 * D], F32)
                gn = k.sb("gn", [2, 2, D], F32)
                k.dma(SP, bm2[:], b_mod[l:l + 1, :].broadcast_to([2, 6 * D]))
                k.dma(SP, gn[:, 0, :], g_n1[l:l + 1, :].broadcast_to([2, D]))
                k.dma(SP, gn[:, 1, :], g_n2[l:l + 1, :].broadcast_to([2, D]))
                wm = [k.sb("wm%d" % i, [128, 8, 512], F32) for i in range(2)]
                pm = [k.ps("pm%d" % i, [128, 512], F32) for i in range(2)]
                wmv = w_mod[l].rearrange("(k p) n -> p k n", p=128)
                for cc in range(12):
                    w_ = wm[cc % 2]
                    k.dma(SP, w_[:], wmv[:, :, cc * 512:(cc + 1) * 512])
                    p_ = pm[cc % 2]
                    for kk in range(8):
                        k.mm(p_[0:2, :], scT[:, kk, :], w_[:, kk, :], start=(kk == 0), stop=(kk == 7))
                    k.tt(DVE, modsb[:, cc * 512:(cc + 1) * 512], p_[0:2, :], bm2[:, cc * 512:(cc + 1) * 512], ALU.add)
                k.stt(DVE, modsb[:, D:2 * D], modsb[:, D:2 * D], 1.0, gn[:, 0, :], ALU.add, ALU.mult)
                k.stt(DVE, modsb[:, 4 * D:5 * D], modsb[:, 4 * D:5 * D], 1.0, gn[:, 1, :], ALU.add, ALU.mult)
                k.dma(SP, modv[l], modsb[:])

            def bc_load(dst, row, slot):
                k.dma(SP, dst, modv[l, row:row + 1, slot * D:(slot + 1) * D].broadcast_to([128, D]))

            with k.phase():
                wi = k.sb("wi", [128, 8, INC], BF16)
                wiv = w_in[l].rearrange("(k p) n -> p k n", p=128)
                for kk in range(8):
                    k.dma(POOL, wi[:, kk, :], wiv[:, kk, :])
                G1 = k.sb("G1", [128, D], F32)
                SH1 = k.sb("SH1", [128, D], F32)
                cur_var = [-1]
                wg2 = k.sb("wg2", [17, 2, 512], BF16)
                for d in range(2):
                    k.dma(POOL, wg2[0:16, d, :], w_g2[l, d])
                    k.dma(POOL, wg2[16:17, d, :], b_g2[l, d:d + 1, :])
                bb = k.sb("bb", [128, 16], F32)
                k.dma(SP, bb[:], bbT[l])
                xt = [k.sb("xt%d" % i, [128, D], F32) for i in range(2)]
                junk = k.sbu("junk", [128, D], BF16)
                tmp = k.sb("tmpA", [128, D], F32)
                hb = k.sb("hb", [128, D], BF16)
                ss = k.sb("ssA", [128, 1], F32)
                rstd = k.sb("rstdA", [128, 1], F32)
                hT = [k.sb("hT%d" % i, [128, 8, 512], BF16) for i in range(2)]
                pT = k.ps("pT", [128, D], BF16)
                pf = [k.ps("pf%d" % i, [128, 512], F32) for i in range(3)]
                pl = k.ps("pl", [16, 512], F32)
                pt = [k.ps("pt%d" % i, [128, 512], F32) for i in range(3)]
                stg = [k.sb("stg%d" % i, [128, 512], BF16) for i in range(4)]
                scnt = [0]
                qTs = k.sb("qTs", [128, 4, 512], BF16)
                kTs = k.sb("kTs", [128, 4, 512], BF16)
                lr1 = [k.sb("lr1_%d" % d, [32, 512], BF16) for d in range(2)]
                for d in range(2):
                    k.memset(DVE, lr1[d][:], 1.0)
                vgs = k.sb("vgs", [128, D], F32)
                vgb = k.sb("vgb", [128, D], BF16)
                vst = k.sb("vst", [128, 2], F32)
                vsb = k.sb("vsb", [128, D], BF16)
                srb = k.sb("srb", [128, D], BF16)
                ktk = k.sb("ktk", [128, 512], BF16)
                sp_ = k.sb("sp_", [128, 512], F32)
                ebt = k.sb("ebt", [128, 4, 128], F32)
                enbt = k.sb("enbt", [128, 4, 128], F32)
                qinb = k.sb("qinb", [128, 4, 128], BF16)
                kinb = k.sb("kinb", [128, 4, 128], BF16)
                edt = k.sb("edt", [128, 512], F32)
                kstb = k.sb("kstb", [128, 512], BF16)
                fcnt = [0]
                tcnt = [0]

                def nxt_pf():
                    fcnt[0] += 1
                    return pf[fcnt[0] % 3]

                def nxt_pt():
                    tcnt[0] += 1
                    return pt[tcnt[0] % 3]

                def xload(tj):
                    xb_ = xt[tj % 2]
                    k.dma(SP, xb_[:], xsrc[tj * 128:(tj + 1) * 128, :])
                    if l == 0:
                        k.dma(SP, xres[tj * 128:(tj + 1) * 128, :], xb_[:])

                for si, (ts0, nts) in enumerate(supers):
                    W = nts * 128
                    c0 = ts0 * 128
                    var = 1 if ts0 < NTC else 0
                    if cur_var[0] != var:
                        bc_load(G1[:], var, 1)
                        bc_load(SH1[:], var, 0)
                        cur_var[0] = var
                    hTs = hT[si % 2]
                    if si == 0:
                        xload(0)
                    for i in range(nts):
                        ti = ts0 + i
                        x_ = xt[ti % 2]
                        if ti + 1 < NT:
                            xload(ti + 1)
                        rms_rstd(x_[:], junk[:], ss[:], rstd[:], D)
                        k.stt(DVE, tmp[:], x_[:], rstd[:, 0:1], G1[:], ALU.mult, ALU.mult)
                        k.tt(POOL, hb[:], tmp[:], SH1[:], ALU.add)
                        for kk in range(8):
                            k.tr(pT[:, kk * 128:(kk + 1) * 128], hb[:, kk * 128:(kk + 1) * 128], identb[:])
                        k.cp(ACT, hTs[:, :, i * 128:(i + 1) * 128], pT[:].rearrange("p (k t) -> p k t", k=8))

                    def fm(col0, M):
                        p_ = nxt_pf() if M == 128 else pl
                        for kk in range(8):
                            k.mm(p_[0:M, 0:W], wi[:, kk, col0:col0 + M], hTs[:, kk, 0:W], start=(kk == 0), stop=(kk == 7))
                        return p_

                    def nxt_stg():
                        scnt[0] += 1
                        return stg[scnt[0] % 4]

                    for j in range(8):
                        p_ = fm(j * 128, 128)
                        s_ = nxt_stg()
                        k.act(ACT, s_[:, 0:W], p_[:, 0:W], AF.Gelu_apprx_tanh)
                        k.dma(SP, uT_d[j, :, c0:c0 + W], s_[:, 0:W])
                    for h in range(4):
                        p_ = fm(2048 + h * 128, 128)
                        k.act(ACT, qTs[:, h, 0:W], p_[:, 0:W], AF.Copy, scale=128.0 ** -0.5)
                        p_ = fm(2560 + h * 128, 128)
                        k.cp(DVE, kTs[:, h, 0:W], p_[:, 0:W])
                    for d in range(2):
                        p_ = fm(5120 + d * 16, 16)
                        k.cp(DVE, lr1[d][0:16, 0:W], p_[0:16, 0:W])
                    for j in range(16):
                        p_ = fm(5152 + j * 128, 128)
                        dst = gaT_d if j < 8 else gbT_d
                        s_ = nxt_stg()
                        k.act(ACT, s_[:, 0:W], p_[:, 0:W], AF.Sigmoid, bias=bb[:, j:j + 1])
                        k.dma(SP, dst[j % 8, :, c0:c0 + W], s_[:, 0:W])

                    for i in range(nts):
                        ti = ts0 + i
                        r0 = ti * 128
                        tsl = slice(i * 128, (i + 1) * 128)

                        def tm(col0, N):
                            p_ = nxt_pt()
                            for kk in range(8):
                                k.mm(p_[:, 0:N], hTs[:, kk, tsl], wi[:, kk, col0:col0 + N], start=(kk == 0), stop=(kk == 7))
                            return p_

                        for hh in range(2):
                            p_ = tm(1024 + hh * 512, 512)
                            k.act(ACT, vgs[:, hh * 512:(hh + 1) * 512], p_[:, :], AF.Gelu_apprx_tanh)
                        k.rsum(vst[:, 0:1], vgs[:])
                        k.ts(DVE, vst[:, 0:1], vst[:, 0:1], -1.0 / D, None, ALU.mult)
                        k.act(ACT, junk[:], vgs[:], AF.Square, bias=vst[:, 0:1], accum_out=vst[:, 1:2])
                        k.act(ACT, vst[:, 1:2], vst[:, 1:2], AF.Ln, bias=epsb[:, 0:1], scale=1.0 / D)
                        k.act(ACT, vst[:, 1:2], vst[:, 1:2], AF.Exp, scale=-0.5)
                        k.ts(DVE, vgb[:], vgs[:], vst[:, 0:1], vst[:, 1:2], ALU.add, ALU.mult)
                        k.dma(SP, vg_d[r0:r0 + 128, :], vgb[:])
                        p_ = tm(2560, 512)
                        k.cp(ACT, ktk[:], p_[:, :])
                        for hh in range(2):
                            p_ = tm(3072 + hh * 512, 512)
                            k.cp(DVE if hh else ACT, vsb[:, hh * 512:(hh + 1) * 512], p_[:, :])
                        k.dma(SP, v_d[r0:r0 + 128, :], vsb[:])
                        for hh in range(2):
                            p_ = tm(4096 + hh * 512, 512)
                            k.act(ACT, srb[:, hh * 512:(hh + 1) * 512], p_[:, :], AF.Silu)
                        k.dma(SP, sr_d[r0:r0 + 128, :], srb[:])
                        for d in range(2):
                            tri = triF if d == 0 else triB
                            tric = triFc if d == 0 else triBc
                            p_ = nxt_pt()
                            k.mm(p_[:, :], lr1[d][0:17, tsl], wg2[0:17, d, :])
                            k.act(ACT, sp_[:], p_[:, :], AF.Exp, scale=-1.0)
                            k.act(ACT, sp_[:], sp_[:], AF.Ln, bias=oneb[:, 0:1])
                            p2 = nxt_pt()
                            for h in range(4):
                                k.mm(p2[:, h * 128:(h + 1) * 128], sp_[:, h * 128:(h + 1) * 128], tri)
                            p2v = p2[:, :].rearrange("p (h t) -> p h t", h=4)
                            k.act(ACT, ebt[:], p2v, AF.Exp, scale=-1.0 / 16)
                            k.act(ACT, enbt[:], p2v, AF.Exp, scale=1.0 / 16)
                            k.tt(DVE, qinb[:], qTs[:, :, tsl], ebt[:], ALU.mult)
                            k.tt(DVE, kinb[:], kTs[:, :, tsl], enbt[:], ALU.mult)
                            k.dma(SP, qin_d[d][:, :, r0:r0 + 128].rearrange("h p t -> p h t"), qinb[:])
                            k.dma(SP, kin_d[d][:, :, r0:r0 + 128].rearrange("h p t -> p h t"), kinb[:])
                            cs = slice(63, 128, 64) if d == 0 else slice(0, 128, 64)
                            k.cp(DVE, decay[:, d, ti, :].rearrange("p (h c) -> p h c", h=4), ebt[:, :, cs])
                            p3 = nxt_pt()
                            k.mm(p3[:, :], tric, sp_[:])
                            k.act(ACT, edt[:], p3[:, :], AF.Exp, scale=-1.0 / 16)
                            k.tt(DVE, kstb[:], ktk[:], edt[:], ALU.mult)
                            k.dma(SP, kst_d[d][r0:r0 + 128, :], kstb[:])

            with k.phase():
                wa = k.sb("wa", [128, 8, D], BF16)
                k.dma(POOL, wa[:], w_a[l].rearrange("(k p) n -> p k n", p=128))
                wsT = k.sb("wsT", [128, 8, 128], BF16)
                k.dma(POOL, wsT[:], sgu_wT[l].rearrange("g s t -> s g t"))
                wsT32 = k.sb("wsT32", [128, 8, 128], F32)
                k.dma(SP, wsT32[:], sgu_wT[l].rearrange("g s t -> s g t"))
                lng = k.sb("lng", [128, 8], F32)
                k.dma(SP, lng[:], ln_gT[l])
                ones32 = k.sb("ones32", [128, 128], F32)
                k.memset(DVE, ones32[:], 1.0)
                R2 = k.sb("R2", [2, 8, 128], F32)
                L2 = k.sb("L2", [2, D], F32)
                k.memset(DVE, L2[:], 1.0)
                k.dma(SP, L2[0:1, :], ln_b[l:l + 1, :])
                k.dma(SP, R2[1:2, :, :], sgu_b[l:l + 1, :, :])
                pmx = [k.ps("pmx%d" % i, [128, D], F32) for i in range(2)]
                prs, pbs = pmx
                for g in range(8):
                    k.mm(prs[0:1, g * 128:(g + 1) * 128], ones32[:, 0:1], wsT32[:, g, :])
                k.cp(DVE, R2[0:1, :, :], prs[0:1, :].rearrange("p (g t) -> p g t", g=8))
                for g in range(8):
                    k.mm(pbs[:, g * 128:(g + 1) * 128], L2[0:2, g * 128:(g + 1) * 128], R2[0:2, g, :])
                Bias = k.sb("Bias", [128, 8, 128], F32)
                k.cp(DVE, Bias[:], pbs[:, :].rearrange("p (g t) -> p g t", g=8))
                vgt = [k.sb("vgt%d" % i, [128, D], BF16) for i in range(2)]
                uTl = [k.sb("uTl%d" % i, [128, 8, 512], BF16) for i in range(2)]
                gaTl = [k.sb("gaTl%d" % i, [128, 8, 512], BF16) for i in range(2)]
                tmpB = k.sb("tmpB", [128, 8, 128], F32)
                gT = k.sb("gT", [128, 8, 512], BF16)
                py = [k.ps("py%d" % i, [128, 512], F32) for i in range(2)]
                yag = k.sb("yag", [128, 8, 512], BF16)
                def sloadB(sj):
                    tsj, ntj = supers[sj]
                    Wj = ntj * 128
                    cj = tsj * 128
                    k.dma(SP, uTl[sj % 2][:, :, 0:Wj], uT_d[:, :, cj:cj + Wj].rearrange("j p t -> p j t"))
                    k.dma(SP, gaTl[sj % 2][:, :, 0:Wj], gaT_d[:, :, cj:cj + Wj].rearrange("j p t -> p j t"))

                def vloadB(tj):
                    k.dma(SP, vgt[tj % 2][:], vg_d[tj * 128:(tj + 1) * 128, :])

                sloadB(0)
                vloadB(0)
                for si, (ts0, nts) in enumerate(supers):
                    W = nts * 128
                    c0 = ts0 * 128
                    uT_ = uTl[si % 2]
                    ga_ = gaTl[si % 2]
                    if si + 1 < len(supers):
                        sloadB(si + 1)
                    for i in range(nts):
                        ti = ts0 + i
                        v_ = vgt[ti % 2]
                        if ti + 1 < NT:
                            vloadB(ti + 1)
                        pm_ = pmx[ti % 2]
                        for g in range(8):
                            k.mm(pm_[:, g * 128:(g + 1) * 128], v_[:, g * 128:(g + 1) * 128], wsT[:, g, :])
                        pv = pm_[:, :].rearrange("p (g t) -> p g t", g=8)
                        k.tt(DVE, tmpB[:], pv, lng[:, :].unsqueeze(2).broadcast_to([128, 8, 128]), ALU.mult)
                        k.tt(POOL, tmpB[:], tmpB[:], Bias[:], ALU.add)
                        k.tt(DVE, gT[:, :, i * 128:(i + 1) * 128], tmpB[:], uT_[:, :, i * 128:(i + 1) * 128], ALU.mult)
                    for oc in range(8):
                        p_ = py[oc % 2]
                        for kk in range(8):
                            k.mm(p_[:, 0:W], wa[:, kk, oc * 128:(oc + 1) * 128], gT[:, kk, 0:W], start=(kk == 0), stop=(kk == 7))
                        k.tt(DVE, yag[:, oc, 0:W], p_[:, 0:W], ga_[:, oc, 0:W], ALU.mult)
                    k.dma(SP, yag_d[:, :, c0:c0 + W].rearrange("j p t -> p j t"), yag[:, :, 0:W])

            with k.phase():
                zer = k.sb("zer", [128, 128], BF16)
                k.memset(DVE, zer[:], 0.0)
                DS = {}
                for d in (1, 0):
                    st = {}
                    if d == 1:
                        st["order"] = list(range(NTC - 1, -1, -1)) + list(range(NT - 1, NTC - 1, -1))
                        st["co"] = (1, 0)
                        st["msk"] = triB
                    else:
                        st["order"] = list(range(NT))
                        st["co"] = (0, 1)
                        st["msk"] = triF
                    st["S32"] = [k.sb("S32_%d_%d" % (d, h), [128, 256], F32) for h in range(4)]
                    st["S16"] = [k.sb("S16_%d_%d" % (d, h), [128, 256], BF16) for h in range(4)]
                    for h in range(4):
                        k.memset(DVE, st["S32"][h][:], 0.0)
                        k.memset(POOL, st["S16"][h][:], 0.0)
                    st["qn"] = [k.sb("qn%d_%d" % (d, i), [128, 4, 128], BF16) for i in range(2)]
                    st["kn"] = [k.sb("kn%d_%d" % (d, i), [128, 4, 128], BF16) for i in range(2)]
                    st["ks"] = [k.sb("ks%d_%d" % (d, i), [128, 512], BF16) for i in range(2)]
                    st["vv"] = [k.sb("vv%d_%d" % (d, i), [128, D], BF16) for i in range(2)]
                    st["patt"] = k.ps("patt%d" % d, [128, 512], F32)
                    st["po"] = k.ps("po%d" % d, [128, D], F32)
                    st["pkv"] = k.ps("pkv%d" % d, [128, 512], F32)
                    st["att"] = k.sb("att%d" % d, [128, 4, 128], BF16)
                    st["osb"] = [k.sb("osb%d_%d" % (d, i), [128, D], F32) for i in range(2)]
                    st["odst"] = ob_d if d == 1 else of_d
                    DS[d] = st

                def loadsC(d, ii):
                    st = DS[d]
                    ti = st["order"][ii]
                    r0 = ti * 128
                    b_ = ii % 2
                    k.dma(SP, st["qn"][b_][:], qin_d[d][:, :, r0:r0 + 128].rearrange("h p t -> p h t"))
                    k.dma(SP, st["kn"][b_][:], kin_d[d][:, :, r0:r0 + 128].rearrange("h p t -> p h t"))
                    k.dma(SP, st["ks"][b_][:], kst_d[d][r0:r0 + 128, :])
                    k.dma(SP, st["vv"][b_][:], v_d[r0:r0 + 128, :])

                def stage0(d, ii):
                    st = DS[d]
                    b_ = ii % 2
                    q_, k_, v_ = st["qn"][b_], st["kn"][b_], st["vv"][b_]
                    pa, po_, at_ = st["patt"], st["po"], st["att"]
                    for h in range(4):
                        k.mm(pa[:, h * 128:(h + 1) * 128], k_[:, h, :], q_[:, h, :])
                    k.tt(DVE, at_[:], pa[:, :].rearrange("p (h t) -> p h t", h=4),
                         st["msk"].unsqueeze(1).broadcast_to([128, 4, 128]), ALU.mult)
                    for hh in range(2):
                        k.mm(po_[:, hh * 512:(hh + 1) * 512], zer[:], v_[:, hh * 512:(hh + 1) * 512], start=True, stop=False)
                    for h in range(4):
                        k.mm(po_[:, h * 256:(h + 1) * 256], at_[:, h, :], v_[:, h * 256:(h + 1) * 256], start=False, stop=False)

                def stage_chunk(d, ii, ci):
                    st = DS[d]
                    b_ = ii % 2
                    ti = st["order"][ii]
                    c = st["co"][ci]
                    q_, ks_, v_ = st["qn"][b_], st["ks"][b_], st["vv"][b_]
                    po_, pkv, S32, S16 = st["po"], st["pkv"], st["S32"], st["S16"]
                    rs = slice(c * 64, (c + 1) * 64)
                    for h in range(4):
                        k.mm(po_[rs, h * 256:(h + 1) * 256], q_[:, h, rs], S16[h][:], start=False, stop=(ci == 1))
                    for hp in range(2):
                        for h in (2 * hp, 2 * hp + 1):
                            k.mm(pkv[:, (h % 2) * 256:(h % 2 + 1) * 256], ks_[rs, h * 128:(h + 1) * 128], v_[rs, h * 256:(h + 1) * 256])
                        for h in (2 * hp, 2 * hp + 1):
                            k.stt(DVE, S32[h][:], S32[h][:], decay[:, d, ti, h * 2 + c:h * 2 + c + 1],
                                  pkv[:, (h % 2) * 256:(h % 2 + 1) * 256], ALU.mult, ALU.add)
                            k.cp(ACT, S16[h][:], S32[h][:])

                def stage3(d, ii):
                    st = DS[d]
                    b_ = ii % 2
                    ti = st["order"][ii]
                    r0 = ti * 128
                    o_ = st["osb"][b_]
                    po_ = st["po"]
                    k.cp(ACT, o_[:, 0:512], po_[:, 0:512])
                    k.cp(DVE, o_[:, 512:D], po_[:, 512:D])
                    k.dma(SP, st["odst"][r0:r0 + 128, :], o_[:])

                for d in (1, 0):
                    loadsC(d, 0)
                for ii in range(NT):
                    for d in (1, 0):
                        if ii + 1 < NT:
                            loadsC(d, ii + 1)
                    for d in (1, 0):
                        stage0(d, ii)
                    for ci in range(2):
                        for d in (1, 0):
                            stage_chunk(d, ii, ci)
                    for d in (1, 0):
                        stage3(d, ii)

            with k.phase():
                GG = k.sb("GG", [128, D], F32)
                k.dma(SP, GG[:], gla_g[l:l + 1, :].broadcast_to([128, D]))
                ofl = [k.sb("ofl%d" % i, [128, D], F32) for i in range(2)]
                obl = [k.sb("obl%d" % i, [128, D], F32) for i in range(2)]
                srl = [k.sb("srl%d" % i, [128, D], BF16) for i in range(2)]
                osum = [k.sb("osum%d" % i, [128, D], F32) for i in range(2)]
                on32 = [k.sb("on32_%d" % i, [128, D], F32) for i in range(2)]
                onb = [k.sb("onb%d" % i, [128, D], BF16) for i in range(2)]
                junkc = k.sbu("junkc", [128, 256], BF16)
                ssc = [k.sb("ssc%d" % i, [128, 4], F32) for i in range(2)]
                pTc = [k.ps("pTc%d" % i, [128, D], BF16) for i in range(2)]
                onTs = [k.sb("onTs%d" % i, [128, 8, 128], BF16) for i in range(2)]
                tilesC = list(range(NTC, NT)) if last else list(range(NT))

                def loadC2(pos):
                    tj = tilesC[pos]
                    rj = tj * 128
                    k.dma(SP, ofl[pos % 2][:], of_d[rj:rj + 128, :])
                    k.dma(SP, obl[pos % 2][:], ob_d[rj:rj + 128, :])
                    k.dma(SP, srl[pos % 2][:], sr_d[rj:rj + 128, :])

                loadC2(0)
                for pos, ti in enumerate(tilesC):
                    b_ = pos % 2
                    r0 = ti * 128
                    if pos + 1 < len(tilesC):
                        loadC2(pos + 1)
                    o_ = osum[b_]
                    k.tt(DVE, o_[:], ofl[b_][:], obl[b_][:], ALU.add)
                    for h in range(4):
                        k.act(ACT, junkc[:], o_[:, h * 256:(h + 1) * 256], AF.Square, accum_out=ssc[b_][:, h:h + 1])
                    k.act(ACT, ssc[b_][:], ssc[b_][:], AF.Ln, bias=epsb[:, 0:1], scale=1.0 / 256)
                    k.act(ACT, ssc[b_][:], ssc[b_][:], AF.Exp, scale=-0.5)
                    k.tt(DVE, on32[b_][:].rearrange("p (h v) -> p h v", h=4), o_[:].rearrange("p (h v) -> p h v", h=4),
                         ssc[b_][:, :].unsqueeze(2).broadcast_to([128, 4, 256]), ALU.mult)
                    k.tt(DVE, on32[b_][:], on32[b_][:], GG[:], ALU.mult)
                    k.tt(POOL, onb[b_][:], on32[b_][:], srl[b_][:], ALU.mult)
                    for kk in range(8):
                        k.tr(pTc[b_][:, kk * 128:(kk + 1) * 128], onb[b_][:, kk * 128:(kk + 1) * 128], identb[:])
                    k.cp(ACT, onTs[b_][:], pTc[b_][:, :].rearrange("p (k t) -> p k t", k=8))
                    k.dma(SP, onT_d[:, :, r0:r0 + 128].rearrange("j p t -> p j t"), onTs[b_][:])

            with k.phase():
                wb_ = k.sb("wb_", [128, 8, D], BF16)
                wo_ = k.sb("wo_", [128, 8, D], BF16)
                k.dma(POOL, wb_[:], w_b[l].rearrange("(k p) n -> p k n", p=128))
                k.dma(POOL, wo_[:], w_out[l].rearrange("(k p) n -> p k n", p=128))
                GT1 = k.sb("GT1", [128, 2, D], F32)
                G2 = k.sb("G2", [128, 2, D], F32)
                SH2 = k.sb("SH2", [128, 2, D], F32)
                for r in range(2):
                    bc_load(GT1[:, r, :], r, 2)
                    bc_load(G2[:, r, :], r, 4)
                    bc_load(SH2[:, r, :], r, 3)
                onl = [k.sb("onl%d" % i, [128, 8, 512], BF16) for i in range(2)]
                yal = [k.sb("yal%d" % i, [128, 8, 512], BF16) for i in range(2)]
                gbl = [k.sb("gbl%d" % i, [128, 8, 512], BF16) for i in range(2)]
                pyb = [k.ps("pyb%d" % i, [128, 512], F32) for i in range(2)]
                tmpD = k.sb("tmpD", [128, 512], F32)
                mT = k.sb("mT", [128, 8, 512], BF16)
                xl = [k.sb("xl%d" % i, [128, D], F32) for i in range(2)]
                pyo = k.ps("pyo", [128, D], F32)
                tmpx = k.sb("tmpx", [128, D], F32)
                xn = [k.sb("xn%d" % i, [128, D], F32) for i in range(2)]
                junkd = k.sbu("junkd", [128, D], F32)
                ssd = k.sb("ssd", [128, 1], F32)
                rsd = k.sb("rsd", [128, 1], F32)
                h2 = k.sb("h2", [128, D], F32)
                pT32 = k.ps("pT32", [128, D], F32)
                h2T32 = k.sb("h2T32", [128, 8, 128], F32)
                h2Ts = [k.sb("h2Ts%d" % i, [128, 8, 512], BF16) for i in range(2)]
                plg = k.ps("plg", [128, NE], F32)
                lg4 = k.sb("lg4", [128, 4, NE], F32)
                pr4 = k.sb("pr4", [128, 4, NE], F32)
                p24 = k.sb("p24", [128, 4, NE], F32)
                eq4 = k.sb("eq4", [128, 4, NE], F32)
                sel4 = k.sb("sel4", [128, 4, NE], F32)
                r4 = k.sb("r4", [128, 4, 4], F32)
                m14 = k.sb("m14", [128, 16], F32)
                m24 = k.sb("m24", [128, 16], F32)
                gs4 = k.sb("gs4", [128, 16], F32)
                gmk4 = k.sb("gmk4", [128, 16], F32)
                lg = k.sb("lg", [128, NE], F32)
                rt = k.sb("rt", [128, 8], F32)
                pr = k.sb("pr", [128, NE], F32)
                p2_ = k.sb("p2_", [128, NE], F32)
                eq = k.sb("eq", [128, NE], F32)
                m1 = k.sb("m1", [128, 4], F32)
                m2 = k.sb("m2", [128, 4], F32)
                gs = k.sb("gs", [128, 4], F32)
                gmk = k.sb("gmk", [128, 4], F32)
                sel = k.sb("sel", [128, NE], F32)

                def g4(ap):
                    return ap.rearrange("p (g e) -> p g e", g=4)

                def b4(ap):
                    return ap.unsqueeze(2).broadcast_to([128, 4, 4])

                supD = [sp for sp in supers if not (last and sp[0] < NTC)]
                tilesD = [ts_ + i_ for (ts_, n_) in supD for i_ in range(n_)]

                def sloadD(sj):
                    tsj, ntj = supD[sj]
                    Wj = ntj * 128
                    cj = tsj * 128
                    k.dma(SP, onl[sj % 2][:, :, 0:Wj], onT_d[:, :, cj:cj + Wj].rearrange("j p t -> p j t"))
                    k.dma(SP, yal[sj % 2][:, :, 0:Wj], yag_d[:, :, cj:cj + Wj].rearrange("j p t -> p j t"))
                    k.dma(SP, gbl[sj % 2][:, :, 0:Wj], gbT_d[:, :, cj:cj + Wj].rearrange("j p t -> p j t"))

                def xloadD(tpos):
                    tj = tilesD[tpos]
                    k.dma(SP, xl[tpos % 2][:], xres[tj * 128:(tj + 1) * 128, :])

                sloadD(0)
                xloadD(0)
                tposD = 0
                for si, (ts0, nts) in enumerate(supD):
                    W = nts * 128
                    c0 = ts0 * 128
                    var = 1 if ts0 < NTC else 0
                    on_, ya_, gb_ = onl[si % 2], yal[si % 2], gbl[si % 2]
                    if si + 1 < len(supD):
                        sloadD(si + 1)
                    for oc in range(8):
                        p_ = pyb[oc % 2]
                        for kk in range(8):
                            k.mm(p_[:, 0:W], wb_[:, kk, oc * 128:(oc + 1) * 128], on_[:, kk, 0:W], start=(kk == 0), stop=(kk == 7))
                        k.tt(DVE, tmpD[:, 0:W], p_[:, 0:W], gb_[:, oc, 0:W], ALU.mult)
                        k.tt(POOL, mT[:, oc, 0:W], tmpD[:, 0:W], ya_[:, oc, 0:W], ALU.add)
                    h2T_ = h2Ts[si % 2]
                    for i in range(nts):
                        ti = ts0 + i
                        r0 = ti * 128
                        x_ = xl[tposD % 2]
                        xn_ = xn[tposD % 2]
                        tposD += 1
                        if tposD < len(tilesD):
                            xloadD(tposD)
                        for hh in range(2):
                            for kk in range(8):
                                k.mm(pyo[:, hh * 512:(hh + 1) * 512], mT[:, kk, i * 128:(i + 1) * 128],
                                     wo_[:, kk, hh * 512:(hh + 1) * 512], start=(kk == 0), stop=(kk == 7))
                        k.tt(DVE, tmpx[:], pyo[:, :], GT1[:, var, :], ALU.mult)
                        k.tt(POOL, xn_[:], tmpx[:], x_[:], ALU.add)
                        k.dma(SP, xres[r0:r0 + 128, :], xn_[:])
                        rms_rstd(xn_[:], junkd[:], ssd[:], rsd[:], D)
                        k.stt(DVE, tmpx[:], xn_[:], rsd[:, 0:1], G2[:, var, :], ALU.mult, ALU.mult)
                        k.tt(POOL, h2[:], tmpx[:], SH2[:, var, :], ALU.add)
                        for kk in range(8):
                            k.tr(pT32[:, kk * 128:(kk + 1) * 128], h2[:, kk * 128:(kk + 1) * 128], ident[:])
                        k.cp(ACT, h2T32[:], pT32[:, :].rearrange("p (k t) -> p k t", k=8))
                        k.cp(DVE, h2T_[:, :, i * 128:(i + 1) * 128], h2T32[:])
                        for kk in range(8):
                            k.mm(plg[:, :], h2T32[:, kk, :], wr[:, kk, :], start=(kk == 0), stop=(kk == 7))
                        k.tt(DVE, lg4[:, i, :], plg[:, :], brt[:], ALU.add)
                    n_ = nts
                    L3 = lg4[:, 0:n_, :]
                    P3 = pr4[:, 0:n_, :]
                    Q3 = p24[:, 0:n_, :]
                    E3 = eq4[:, 0:n_, :]
                    S3 = sel4[:, 0:n_, :]

                    def ge(ap):
                        return ap.rearrange("p j (g e) -> p (j g) e", g=4)

                    def bl(ap, m):
                        return ap.unsqueeze(2).broadcast_to([128, ap.shape[1], m])

                    k.I(DVE, lambda e: e.tensor_reduce(out=r4[:, 0, 0:n_], in_=L3, axis=AX.X, op=ALU.max), [r4[:]], [lg4[:]])
                    k.tt(DVE, L3, L3, bl(r4[:, 0, 0:n_], NE), ALU.subtract)
                    k.act(ACT, P3, L3, AF.Exp)
                    k.I(DVE, lambda e: e.tensor_reduce(out=r4[:, 1, 0:n_], in_=P3, axis=AX.X, op=ALU.add), [r4[:]], [pr4[:]])
                    k.recip(r4[:, 1, 0:n_], r4[:, 1, 0:n_])
                    k.tt(DVE, P3, P3, bl(r4[:, 1, 0:n_], NE), ALU.mult)
                    k.I(DVE, lambda e: e.tensor_reduce(out=m14[:, 0:4 * n_], in_=ge(P3), axis=AX.X, op=ALU.max), [m14[:]], [pr4[:]])
                    k.tt(DVE, ge(E3), ge(P3), bl(m14[:, 0:4 * n_], 4), ALU.is_ge)
                    k.stt(DVE, Q3, E3, -4.0, P3, ALU.mult, ALU.add)
                    k.I(DVE, lambda e: e.tensor_reduce(out=m24[:, 0:4 * n_], in_=ge(Q3), axis=AX.X, op=ALU.max), [m24[:]], [p24[:]])
                    k.tt(DVE, gs4[:, 0:4 * n_], m14[:, 0:4 * n_], m24[:, 0:4 * n_], ALU.add)
                    gsv = gs4[:, 0:4 * n_].rearrange("p (j g) -> p j g", g=4)
                    k.I(DVE, lambda e: e.tensor_reduce(out=r4[:, 2, 0:n_], in_=gsv, axis=AX.X, op=ALU.max), [r4[:]], [gs4[:]])
                    k.tt(DVE, gmk4[:, 0:4 * n_].rearrange("p (j g) -> p j g", g=4), gsv, bl(r4[:, 2, 0:n_], 4), ALU.is_ge)
                    k.tt(DVE, ge(S3), ge(P3), bl(m24[:, 0:4 * n_], 4), ALU.is_ge)
                    k.tt(DVE, ge(S3), ge(S3), bl(gmk4[:, 0:4 * n_], 4), ALU.mult)
                    k.tt(DVE, S3, S3, P3, ALU.mult)
                    k.I(DVE, lambda e: e.tensor_reduce(out=r4[:, 3, 0:n_], in_=S3, axis=AX.X, op=ALU.add), [r4[:]], [sel4[:]])
                    k.recip(r4[:, 3, 0:n_], r4[:, 3, 0:n_])
                    k.tt(DVE, wts[:, ts0:ts0 + n_, :], S3, bl(r4[:, 3, 0:n_], NE), ALU.mult)
                    k.dma(SP, h2T_d[:, :, c0:c0 + W].rearrange("j p t -> p j t"), h2T_[:, :, 0:W])

            moe_tiles = list(range(NTC, NT)) if last else list(range(NT))
            GSZ = 17
            groups = [moe_tiles[i:i + GSZ] for i in range(0, len(moe_tiles), GSZ)]
            with k.phase():
                GT2 = k.sb("GT2", [128, 2, D], F32)
                for r in range(2):
                    bc_load(GT2[:, r, :], r, 5)
                if last:
                    GF = k.sb("GF", [128, D], F32)
                    k.dma(SP, GF[:], g_fin.broadcast_to([128, D]))
                hT2 = k.sb("hT2", [128, 8, GSZ * 128], BF16)
                yacc = k.sb("yacc", [128, GSZ, D], F32)
                wg_ = [k.sb("wg_%d" % i, [128, 8, 512], BF16) for i in range(2)]
                wu_ = [k.sb("wu_%d" % i, [128, 8, 512], BF16) for i in range(2)]
                wd_ = [k.sb("wd_%d" % i, [128, 4, D], BF16) for i in range(2)]
                pg = [k.ps("pg%d" % i, [128, 512], F32) for i in range(2)]
                pu = [k.ps("pu%d" % i, [128, 512], F32) for i in range(2)]
                pd = [k.ps("pd%d" % i, [128, 512], F32) for i in range(4)]
                sg = [k.sb("sg%d" % i, [128, 512], F32) for i in range(2)]
                aT = [k.sb("aT%d" % i, [128, 4, 512], BF16) for i in range(2)]
                xe = [k.sb("xe%d" % i, [128, D], F32) for i in range(2)]
                tmpe = k.sb("tmpe", [128, D], F32)
                junke = k.sbu("junke", [128, D], F32)
                sse = k.sb("sse", [128, 1], F32)
                rse = k.sb("rse", [128, 1], F32)
                ucnt = 0
                dcnt = 0
                for grp in groups:
                    ng = len(grp)
                    g0 = grp[0] * 128
                    WG = ng * 128
                    k.dma(SP, hT2[:, :, 0:WG], h2T_d[:, :, g0:g0 + WG].rearrange("j p t -> p j t"))
                    k.memset(POOL, yacc[:, 0:ng, :], 0.0)
                    gsup = [(s, min(4, ng - s)) for s in range(0, ng, 4)]
                    units = [(e, q) for e in range(NE) for q in range(3)]

                    def load_unit(ui, buf):
                        e, q = units[ui]
                        k.dma(POOL, wg_[buf][:], w_eg[l, e].rearrange("(k p) n -> p k n", p=128)[:, :, q * 512:(q + 1) * 512])
                        k.dma(POOL, wu_[buf][:], w_eu[l, e].rearrange("(k p) n -> p k n", p=128)[:, :, q * 512:(q + 1) * 512])
                        k.dma(POOL, wd_[buf][:], w_ed[l, e, q * 512:(q + 1) * 512, :].rearrange("(j p) n -> p j n", p=128))

                    load_unit(0, ucnt % 2)
                    for ui, (e, q) in enumerate(units):
                        ub = ucnt % 2
                        ucnt += 1
                        if ui + 1 < len(units):
                            load_unit(ui + 1, ucnt % 2)
                        for sidx, (s0, sn) in enumerate(gsup):
                            W = sn * 128
                            a_ = aT[sidx % 2]
                            for jj in range(4):
                                pg_ = pg[jj % 2]
                                pu_ = pu[jj % 2]
                                for kk in range(8):
                                    k.mm(pg_[:, 0:W], wg_[ub][:, kk, jj * 128:(jj + 1) * 128], hT2[:, kk, s0 * 128:s0 * 128 + W],
                                         start=(kk == 0), stop=(kk == 7))
                                for kk in range(8):
                                    k.mm(pu_[:, 0:W], wu_[ub][:, kk, jj * 128:(jj + 1) * 128], hT2[:, kk, s0 * 128:s0 * 128 + W],
                                         start=(kk == 0), stop=(kk == 7))
                                sg_ = sg[jj % 2]
                                k.act(ACT, sg_[:, 0:W], pg_[:, 0:W], AF.Silu)
                                k.tt(DVE, a_[:, jj, 0:W], sg_[:, 0:W], pu_[:, 0:W], ALU.mult)
                            for i in range(sn):
                                tl = s0 + i
                                ti = grp[tl]
                                for hh in range(2):
                                    pd_ = pd[dcnt % 4]
                                    dcnt += 1
                                    for jj in range(4):
                                        k.mm(pd_[:, :], a_[:, jj, i * 128:(i + 1) * 128], wd_[ub][:, jj, hh * 512:(hh + 1) * 512],
                                             start=(jj == 0), stop=(jj == 3))
                                    k.stt(DVE, yacc[:, tl, hh * 512:(hh + 1) * 512], pd_[:, :], wts[:, ti, e:e + 1],
                                          yacc[:, tl, hh * 512:(hh + 1) * 512], ALU.mult, ALU.add)
                    for tl, ti in enumerate(grp):
                        r0 = ti * 128
                        var = 1 if ti < NTC else 0
                        x_ = xe[tl % 2]
                        k.dma(SP, x_[:], xres[r0:r0 + 128, :])
                        k.tt(POOL, tmpe[:], yacc[:, tl, :], GT2[:, var, :], ALU.mult)
                        k.tt(DVE, x_[:], tmpe[:], x_[:], ALU.add)
                        if not last:
                            k.dma(SP, xres[r0:r0 + 128, :], x_[:])
                        else:
                            rms_rstd(x_[:], junke[:], sse[:], rse[:], D)
                            k.stt(DVE, x_[:], x_[:], rse[:, 0:1], GF[:], ALU.mult, ALU.mult)
                            k.dma(SP, out[(ti - NTC) * 128:(ti - NTC + 1) * 128, :], x_[:])
    return nc


def _consts():
    ident = np.eye(128, dtype=np.float32)
    s = np.arange(128)[:, None]
    t = np.arange(128)[None, :]
    same = (s // 64) == (t // 64)
    triF = (same & (s <= t)).astype(np.float32)
    triFc = (same & (s > t)).astype(np.float32)
    triB = (same & (s >= t)).astype(np.float32)
    triBc = (same & (s < t)).astype(np.float32)
    return ident, np.stack([triF, triFc, triB, triBc])


def make_in_maps(inp, n_cores):
    f = lambda a: np.ascontiguousarray(np.asarray(a, dtype=np.float32))
    L = inp["w_mod"].shape[0]
    ident, masks = _consts()
    shared = {
        "w_mod": f(inp["w_mod"]), "b_mod": f(inp["b_mod"]), "g_norm1": f(inp["g_norm1"]), "g_norm2": f(inp["g_norm2"]),
        "w_in": f(inp["w_in"]), "w_gate2": f(inp["w_gate2"]), "b_gate2": f(inp["b_gate2"]),
        "gla_norm_g": f(inp["gla_norm_g"]),
        "ln_gT": f(np.asarray(inp["sgu_ln_g"]).reshape(L, 8, 128).transpose(0, 2, 1)),
        "sgu_ln_b": f(inp["sgu_ln_b"]),
        "sgu_wT": f(np.asarray(inp["sgu_w"]).transpose(0, 1, 3, 2)),
        "sgu_b": f(inp["sgu_b"]),
        "w_branch_a": f(inp["w_branch_a"]), "w_branch_b": f(inp["w_branch_b"]),
        "b_branchT": f(np.asarray(inp["b_branch"]).reshape(L, 16, 128).transpose(0, 2, 1)),
        "w_out": f(inp["w_out"]), "w_router": f(inp["w_router"]),
        "b_router": f(np.asarray(inp["b_router"]).reshape(1, NE)),
        "w_exp_gate": f(inp["w_exp_gate"]), "w_exp_up": f(inp["w_exp_up"]), "w_exp_down": f(inp["w_exp_down"]),
        "g_final": f(np.asarray(inp["g_final"]).reshape(1, D)),
        "ident": ident, "masks": masks,
    }
    x = np.asarray(inp["x"], dtype=np.float32)
    ctx = np.asarray(inp["ctx"], dtype=np.float32)
    c = np.asarray(inp["c"], dtype=np.float32)
    c_ctx = np.asarray(inp["c_ctx"], dtype=np.float32)
    maps = []
    for b in range(n_cores):
        m = dict(shared)
        m["xin"] = np.ascontiguousarray(np.concatenate([ctx[b], x[b]], axis=0))
        c2 = np.stack([c[b], c_ctx], axis=0)
        m["cT"] = np.ascontiguousarray(c2.reshape(2, 8, 128).transpose(2, 1, 0))[None]
        maps.append(m)
    return maps


_NC_CACHE = {}


def kernel(**inputs):
    x = np.asarray(inputs["x"])
    B, S, _ = x.shape
    C = np.asarray(inputs["ctx"]).shape[1]
    key = (C // 128, S // 128)
    if key not in _NC_CACHE:
        _NC_CACHE[key] = build(NTC=C // 128, NTL=S // 128, L=int(np.asarray(inputs["w_mod"]).shape[0]), NB=1)
    nc = _NC_CACHE[key]
    maps = make_in_maps(inputs, B)
    res = run_bass_kernel_spmd(nc, maps, core_ids=list(range(B)))
    return np.stack([np.asarray(r["out"]).reshape(S, D) for r in res.results], axis=0).astype(np.float32)
```
